# Optimizing a Trainium2 kernel written in Bass

```python
import math
import jax, jax.numpy as jnp
from jax import lax
import numpy as np

D_MODEL = 2048
BATCH = 16
SEQ = 256
DEPTH = 1
DEC_BATCH = 8
DEC_SEQ = 1024
PAST_LEN = 256

GRID_W = 64
EPS = 1e-6
S5_WIDTH = 1024
S5_GROUP = 16
S5_GROUPS = S5_WIDTH // S5_GROUP
S5_STATE = 64
RET_HEADS = 8
RET_DK = 128
RET_DV = 256
RET_QK = RET_HEADS * RET_DK
RET_V = RET_HEADS * RET_DV
RET_CHUNK = 128
ROPE_BASE = 10000.0
N_EXPERTS = 16
EXPERT_FF = 4096
CAPACITY_FACTOR = 2
IN_COLS = S5_WIDTH + 2 * RET_QK + 2 * RET_V + 2 * D_MODEL
SPLITS = (S5_WIDTH, S5_WIDTH + RET_QK, S5_WIDTH + 2 * RET_QK, S5_WIDTH + 2 * RET_QK + RET_V, S5_WIDTH + 2 * RET_QK + 2 * RET_V, S5_WIDTH + 2 * RET_QK + 2 * RET_V + D_MODEL)

kernel_name = 'hybrid_s5_retention_ecmoe_diffusion_step'

F32 = jnp.float32


def _rmsnorm(x, w):
    xf = x.astype(F32)
    y = xf * lax.rsqrt(jnp.mean(xf * xf, axis=-1, keepdims=True) + EPS)
    return (y * w.astype(F32)).astype(x.dtype)


def _adaln(cvec, w_ada, b_ada):
    m = jax.nn.silu(cvec) @ w_ada + b_ada
    return jnp.split(m[..., None, :], 6, axis=-1)


def _rope(x, pos):
    half = x.shape[-1] // 2
    freqs = ROPE_BASE ** (-jnp.arange(half, dtype=F32) / half)
    ang = pos.astype(F32)[:, None] * freqs[None, :]
    cos = jnp.cos(ang)[None, :, None, :]
    sin = jnp.sin(ang)[None, :, None, :]
    x1, x2 = x[..., :half], x[..., half:]
    return jnp.concatenate([x1 * cos - x2 * sin, x1 * sin + x2 * cos], axis=-1)


def _grid_rope(x):
    t = x.shape[1]
    rows = t // GRID_W
    row = jnp.repeat(jnp.arange(rows), GRID_W)
    col = jnp.tile(jnp.arange(GRID_W), rows)
    h = x.shape[-1] // 2
    return jnp.concatenate([_rope(x[..., :h], row), _rope(x[..., h:], col)], axis=-1)


def _affine_combine(e1, e2):
    a1, b1 = e1
    a2, b2 = e2
    return (a2 * a1, a2 * b1 + b2)


def _s5_direction(u, a_dt, b_bar, c_mat, h0, reverse):
    t = u.shape[1]
    bu = jnp.einsum('gph,btgh->btgp', b_bar, u)
    a_seq = jnp.broadcast_to(jnp.exp(a_dt), bu.shape)
    _, h = lax.associative_scan(_affine_combine, (a_seq, bu), reverse=reverse, axis=1)
    if h0 is not None:
        steps = jnp.arange(1, t + 1, dtype=F32)
        if reverse:
            steps = steps[::-1]
        h = h + jnp.exp(a_dt[None] * steps[:, None, None])[None] * h0[:, None]
    y = jnp.einsum('ghp,btgp->btgh', c_mat, h).real
    h_last = h[:, 0] if reverse else h[:, -1]
    return y, h_last


def _s5_branch(u, a_re, a_im, log_dt, b_re, b_im, c_re, c_im, d_skip, w_glu, h0):
    bsz, t, _ = u.shape
    uf = u.astype(F32).reshape(bsz, t, S5_GROUPS, S5_GROUP)
    a = lax.complex(a_re.astype(F32), a_im.astype(F32))
    a_dt = a * jnp.exp(log_dt.astype(F32))[..., None]
    b_bar = ((jnp.exp(a_dt) - 1.0) / a)[..., None] * lax.complex(b_re.astype(F32), b_im.astype(F32))
    c_mat = lax.complex(c_re.astype(F32), c_im.astype(F32))
    uc = uf.astype(jnp.complex64)
    h0_f = None if h0 is None else h0[:, 0]
    h0_b = None if h0 is None else h0[:, 1]
    y_f, h_f = _s5_direction(uc, a_dt[0], b_bar[0], c_mat[0], h0_f, False)
    y_b, h_b = _s5_direction(uc, a_dt[1], b_bar[1], c_mat[1], h0_b, True)
    y = (y_f + y_b + uf * d_skip.astype(F32).reshape(S5_GROUPS, S5_GROUP)).reshape(bsz, t, S5_WIDTH)
    z = jax.nn.gelu(y).astype(u.dtype) @ w_glu
    za, zb = jnp.split(z, 2, axis=-1)
    return za * jax.nn.sigmoid(zb), jnp.stack([h_f, h_b], axis=1)


def _retention_chunkwise(q, k, v, log_gamma, s0, inclusive):
    bsz, t, nh, _ = q.shape
    dv = v.shape[-1]
    n = t // RET_CHUNK

    def to_chunks(z):
        return jnp.moveaxis(z.reshape(bsz, n, RET_CHUNK, nh, z.shape[-1]), 1, 0)

    idx = jnp.arange(RET_CHUNK, dtype=F32)
    diff = idx[:, None] - idx[None, :]
    mask = (diff >= 0) if inclusive else (diff > 0)
    intra = jnp.where(mask[None], jnp.exp(log_gamma[:, None, None] * jnp.where(mask, diff, 0.0)[None]), 0.0)
    q_dec = jnp.exp(log_gamma[None, :] * (idx + 1.0)[:, None])[None, :, :, None]
    k_dec = jnp.exp(log_gamma[None, :] * (RET_CHUNK - 1.0 - idx)[:, None])[None, :, :, None]
    chunk_dec = jnp.exp(log_gamma * RET_CHUNK)[None, :, None, None]

    def step(s, qkv):
        qc, kc, vc = qkv
        scores = jnp.einsum('bihd,bjhd->bhij', qc, kc) * intra[None]
        inner = jnp.einsum('bhij,bjhe->bihe', scores, vc)
        cross = jnp.einsum('bihd,bhde->bihe', qc, s) * q_dec
        s_new = s * chunk_dec + jnp.einsum('bjhd,bjhe->bhde', kc * k_dec, vc)
        return s_new, inner + cross

    s_fin, out = lax.scan(step, s0, (to_chunks(q), to_chunks(k), to_chunks(v)))
    return jnp.moveaxis(out, 0, 1).reshape(bsz, t, nh, dv), s_fin


def _retention_branch(q, k, v, g, decay_logit, gn_w, w_ret_out, s0, latent):
    bsz, t, _ = q.shape
    qf = q.astype(F32).reshape(bsz, t, RET_HEADS, RET_DK) * (RET_DK ** -0.5)
    kf = k.astype(F32).reshape(bsz, t, RET_HEADS, RET_DK)
    vf = v.astype(F32).reshape(bsz, t, RET_HEADS, RET_DV)
    if latent:
        qf = _grid_rope(qf)
        kf = _grid_rope(kf)
    log_gamma = jax.nn.log_sigmoid(decay_logit.astype(F32))
    if s0 is None:
        s0 = jnp.zeros((bsz, 2, RET_HEADS, RET_DK, RET_DV), F32)
    o_f, s_f = _retention_chunkwise(qf, kf, vf, log_gamma[0], s0[:, 0], True)
    o_b, s_b = _retention_chunkwise(jnp.flip(qf, 1), jnp.flip(kf, 1), jnp.flip(vf, 1), log_gamma[1], s0[:, 1], False)
    o = o_f + jnp.flip(o_b, 1)
    mu = jnp.mean(o, axis=-1, keepdims=True)
    var = jnp.mean(jnp.square(o - mu), axis=-1, keepdims=True)
    o = ((o - mu) * lax.rsqrt(var + EPS)).reshape(bsz, t, RET_V) * gn_w.astype(F32)
    o = (jax.nn.silu(g.astype(F32)) * o).astype(q.dtype)
    return o @ w_ret_out, jnp.stack([s_f, s_b], axis=1)


def _expert_choice_ffn(h, w_router, w_gate, w_up, w_down):
    bsz, t, d = h.shape
    cap = CAPACITY_FACTOR * t // N_EXPERTS
    logits = jnp.einsum('btd,de->bte', h.astype(F32), w_router.astype(F32))
    affinity = jax.nn.softmax(logits, axis=-1)
    gate, idx = lax.top_k(jnp.swapaxes(affinity, 1, 2), cap)
    xs = jax.vmap(lambda hb, ib: hb[ib])(h, idx)
    hid = jax.nn.silu(jnp.einsum('becd,edf->becf', xs, w_gate)) * jnp.einsum('becd,edf->becf', xs, w_up)
    out = jnp.einsum('becf,efd->becd', hid, w_down) * gate[..., None].astype(h.dtype)
    return jax.vmap(lambda ob, ib: jnp.zeros((t, d), h.dtype).at[ib.reshape(-1)].add(ob.reshape(-1, d)))(out, idx)


def _layer(x, mod, latent, s5_h0, ret_s0, p, l):
    shift1, scale1, gate1, shift2, scale2, gate2 = mod
    h = (_rmsnorm(x, p['norm1'][l]) * (1.0 + scale1) + shift1).astype(x.dtype)
    u, q, k, v, g, ga, gb = jnp.split(h @ p['w_in'][l], SPLITS, axis=-1)
    out_a, s5_fin = _s5_branch(u, p['s5_a_re'][l], p['s5_a_im'][l], p['s5_log_dt'][l], p['s5_b_re'][l], p['s5_b_im'][l], p['s5_c_re'][l], p['s5_c_im'][l], p['s5_d'][l], p['w_s5_glu'][l], s5_h0)
    out_b, ret_fin = _retention_branch(q, k, v, g, p['ret_decay_logit'][l], p['ret_gn_w'][l], p['w_ret_out'][l], ret_s0, latent)
    merged = jax.nn.sigmoid(ga) * out_a + jax.nn.sigmoid(gb) * out_b
    x = (x + gate1 * (merged @ p['w_out'][l])).astype(x.dtype)
    h2 = (_rmsnorm(x, p['norm2'][l]) * (1.0 + scale2) + shift2).astype(x.dtype)
    x = (x + gate2 * _expert_choice_ffn(h2, p['w_router'][l], p['w_exp_gate'][l], p['w_exp_up'][l], p['w_exp_down'][l])).astype(x.dtype)
    return x, s5_fin, ret_fin


def setup_inputs(seed: int = 0) -> dict:
    key = jax.random.key(seed)
    ks = jax.random.split(key, 32)

    def nrm(k, shape, s):
        return jax.random.normal(k, shape, F32) * s

    G, P, HG = S5_GROUPS, S5_STATE, S5_GROUP
    n_idx = jnp.arange(P, dtype=F32)
    ret_logit0 = jnp.log(2.0 ** (5.0 + jnp.arange(RET_HEADS, dtype=F32)) - 1.0)
    return {
        'x_prompt': nrm(ks[0], (BATCH, SEQ, D_MODEL), 1.0),
        'x_sample': nrm(ks[1], (DEC_BATCH, DEC_SEQ, D_MODEL), 1.0),
        'state_s5_re': nrm(ks[2], (DEC_BATCH, DEPTH, 2, G, P), 0.1),
        'state_s5_im': nrm(ks[3], (DEC_BATCH, DEPTH, 2, G, P), 0.1),
        'state_ret': nrm(ks[4], (DEC_BATCH, DEPTH, 2, RET_HEADS, RET_DK, RET_DV), 1.0),
        'c': nrm(ks[5], (DEC_BATCH, D_MODEL), 1.0),
        'c_ctx': nrm(ks[6], (D_MODEL,), 1.0),
        'final_norm': 1.0 + nrm(ks[7], (D_MODEL,), 0.01),
        'w_ada': nrm(ks[8], (DEPTH, D_MODEL, 6 * D_MODEL), 0.5 * D_MODEL ** -0.5),
        'b_ada': nrm(ks[9], (DEPTH, 6 * D_MODEL), 0.01),
        'norm1': 1.0 + nrm(ks[10], (DEPTH, D_MODEL), 0.01),
        'norm2': 1.0 + nrm(ks[11], (DEPTH, D_MODEL), 0.01),
        'w_in': nrm(ks[12], (DEPTH, D_MODEL, IN_COLS), D_MODEL ** -0.5),
        's5_a_re': -0.5 + nrm(ks[13], (DEPTH, 2, G, P), 0.01),
        's5_a_im': math.pi * n_idx + nrm(ks[14], (DEPTH, 2, G, P), 0.01),
        's5_log_dt': jax.random.uniform(ks[15], (DEPTH, 2, G), F32, math.log(1e-3), math.log(1e-1)),
        's5_b_re': nrm(ks[16], (DEPTH, 2, G, P, HG), (2.0 * HG) ** -0.5),
        's5_b_im': nrm(ks[17], (DEPTH, 2, G, P, HG), (2.0 * HG) ** -0.5),
        's5_c_re': nrm(ks[18], (DEPTH, 2, G, HG, P), (2.0 * P) ** -0.5),
        's5_c_im': nrm(ks[19], (DEPTH, 2, G, HG, P), (2.0 * P) ** -0.5),
        's5_d': nrm(ks[20], (DEPTH, S5_WIDTH), 1.0),
        'w_s5_glu': nrm(ks[21], (DEPTH, S5_WIDTH, 2 * D_MODEL), S5_WIDTH ** -0.5),
        'ret_decay_logit': ret_logit0 + nrm(ks[22], (DEPTH, 2, RET_HEADS), 0.01),
        'ret_gn_w': 1.0 + nrm(ks[23], (DEPTH, RET_V), 0.01),
        'w_ret_out': nrm(ks[24], (DEPTH, RET_V, D_MODEL), RET_V ** -0.5),
        'w_out': nrm(ks[25], (DEPTH, D_MODEL, D_MODEL), D_MODEL ** -0.5),
        'w_router': nrm(ks[26], (DEPTH, D_MODEL, N_EXPERTS), D_MODEL ** -0.5),
        'w_exp_gate': nrm(ks[27], (DEPTH, N_EXPERTS, D_MODEL, EXPERT_FF), D_MODEL ** -0.5),
        'w_exp_up': nrm(ks[28], (DEPTH, N_EXPERTS, D_MODEL, EXPERT_FF), D_MODEL ** -0.5),
        'w_exp_down': nrm(ks[29], (DEPTH, N_EXPERTS, EXPERT_FF, D_MODEL), EXPERT_FF ** -0.5),
    }


def reference(x_prompt, x_sample, state_s5_re, state_s5_im, state_ret, c, c_ctx, final_norm, w_ada, b_ada, norm1, norm2, w_in, s5_a_re, s5_a_im, s5_log_dt, s5_b_re, s5_b_im, s5_c_re, s5_c_im, s5_d, w_s5_glu, ret_decay_logit, ret_gn_w, w_ret_out, w_out, w_router, w_exp_gate, w_exp_up, w_exp_down):
    p = {'norm1': norm1, 'norm2': norm2, 'w_in': w_in, 's5_a_re': s5_a_re, 's5_a_im': s5_a_im, 's5_log_dt': s5_log_dt, 's5_b_re': s5_b_re, 's5_b_im': s5_b_im, 's5_c_re': s5_c_re, 's5_c_im': s5_c_im, 's5_d': s5_d, 'w_s5_glu': w_s5_glu, 'ret_decay_logit': ret_decay_logit, 'ret_gn_w': ret_gn_w, 'w_ret_out': w_ret_out, 'w_out': w_out, 'w_router': w_router, 'w_exp_gate': w_exp_gate, 'w_exp_up': w_exp_up, 'w_exp_down': w_exp_down}
    xp = x_prompt
    xs = x_sample
    new_re, new_im, new_ret = [], [], []
    for l in range(DEPTH):
        mod_ctx = _adaln(c_ctx, w_ada[l], b_ada[l])
        mod_lat = _adaln(c, w_ada[l], b_ada[l])
        xp, s5_fin, ret_fin = _layer(xp, mod_ctx, False, None, None, p, l)
        new_re.append(s5_fin.real)
        new_im.append(s5_fin.imag)
        new_ret.append(ret_fin)
        h0 = lax.complex(state_s5_re[:, l].astype(F32), state_s5_im[:, l].astype(F32))
        xs, _, _ = _layer(xs, mod_lat, True, h0, state_ret[:, l].astype(F32), p, l)
    y_prompt = _rmsnorm(xp, final_norm)
    y_sample = _rmsnorm(xs, final_norm)
    return (y_prompt, y_sample, jnp.stack(new_re, axis=1), jnp.stack(new_im, axis=1), jnp.stack(new_ret, axis=1))
```

```python
import numpy as np
import concourse.bass as bass
import concourse.mybir as mybir
from concourse.bass_utils import run_bass_kernel_spmd

F32 = mybir.dt.float32
BF16 = mybir.dt.bfloat16
ALU = mybir.AluOpType
AF = mybir.ActivationFunctionType
AX = mybir.AxisListType

D = 2048
NCORES = 8
EPS = 1e-6
S5W = 1024
NG = 64
NP_ = 64
NH = 8
DK = 128
DV = 256
NE = 16
FF = 4096
IN_COLS = 11264
C_U, C_Q, C_K, C_V, C_G, C_GA, C_GB = 0, 1024, 2048, 3072, 5120, 7168, 9216


class Reg:
    __slots__ = ("name", "w", "r", "sem", "cnt")

    def __init__(self, name):
        self.name = name
        self.w = None
        self.r = {}
        self.sem = None
        self.cnt = 0


class K:
    def __init__(self, nc):
        self.nc = nc
        self.eng = {"pe": nc.tensor, "dve": nc.vector, "act": nc.scalar, "pool": nc.gpsimd, "sp": nc.sync}
        self.sem = {}
        self.cnt = {}
        self.seen = {e: {} for e in self.eng}
        self.nsem = 0
        self.dma_toks = {}
        for e in self.eng:
            self._newsem(e)

    def _mk(self, name):
        self.nsem += 1
        return self.nc.semaphore(f"{name}_{self.nsem}").__enter__()

    def _newsem(self, e):
        self.sem[e] = self._mk(e)
        self.cnt[e] = 0

    def _wait(self, e, tok):
        if tok is None:
            return
        sem, val, src = tok
        if src == "pe" and e == "pe":
            return
        d = self.seen[e]
        key = id(sem)
        if d.get(key, (None, 0))[1] >= val:
            return
        self.eng[e].wait_ge(sem, val)
        d[key] = (sem, val)

    def _deps(self, e, reads, writes):
        for r in reads:
            self._wait(e, r.w)
        for r in writes:
            self._wait(e, r.w)
            for t in r.r.values():
                self._wait(e, t)

    def _commit(self, tok, reads, writes):
        for r in writes:
            r.w = tok
            r.r = {}
        for r in reads:
            r.r[tok[2]] = tok

    def op(self, e, fn, reads=(), writes=()):
        self._deps(e, reads, writes)
        ins = fn(self.eng[e])
        if self.cnt[e] > 30000:
            self._newsem(e)
        self.cnt[e] += 1
        ins.then_inc(self.sem[e], 1)
        tok = (self.sem[e], self.cnt[e], e)
        self._commit(tok, reads, writes)
        return tok

    def dma(self, e, out, in_, reads=(), writes=()):
        self._deps(e, reads, writes)
        r0 = writes[0]
        if r0.sem is None or r0.cnt > 1800:
            r0.sem = self._mk("d")
            r0.cnt = 0
        r0.cnt += 1
        self.eng[e].dma_start(out=out, in_=in_).then_inc(r0.sem, 16)
        tok = (r0.sem, 16 * r0.cnt, "dma_" + e + r0.name)
        self.dma_toks[id(r0.sem)] = tok
        self._commit(tok, reads, writes)
        return tok

    def barrier(self):
        toks = [(self.sem[e], self.cnt[e], e) for e in self.eng if self.cnt[e] > 0]
        toks += list(self.dma_toks.values())
        for e in self.eng:
            for t in toks:
                if t[2] == e:
                    continue
                sem, val, src = t
                d = self.seen[e]
                if d.get(id(sem), (None, 0))[1] >= val:
                    continue
                self.eng[e].wait_ge(sem, val)
                d[id(sem)] = (sem, val)

    def finish(self, regs):
        for r in regs:
            self._wait("sp", r.w)


def build(cfg):
    nc = bass.Bass("TRN2", target_bir_lowering=False)
    k = K(nc)
    reqs = cfg["reqs"]
    NTOK = sum(r[0] for r in reqs)
    NCV = cfg["ncv"]
    NEXP = cfg.get("nexp", NE)
    dbg = cfg.get("dbg", {})
    stop = cfg.get("stop")
    dbg_req = cfg.get("dbg_req", 0)

    def din(name, shape, dt=F32):
        return nc.dram_tensor(name, list(shape), dt, kind="ExternalInput").ap()

    def dout(name, shape, dt=F32):
        return nc.dram_tensor(name, list(shape), dt, kind="ExternalOutput").ap()

    def sb(name, shape, dt=F32):
        return nc.sbuf_tensor(name, list(shape), dt).__enter__()

    def ps(name, shape, dt=F32):
        return nc.psum_tensor(name, list(shape), dt).__enter__()

    xin = din("xin", [NTOK, D])
    cT_d = din("cT", [128, 16 * NCV])
    ident_d = din("ident", [128, 128])
    w_ada = din("w_ada", [D, 6 * D])
    b_ada = din("b_ada", [6 * D])
    norm1_d = din("norm1", [D])
    norm2_d = din("norm2", [D])
    fnorm_d = din("final_norm", [D])
    w_in = din("w_in", [D, IN_COLS])
    yout = dout("y", [NTOK, D])
    dbg_outs = {}
    for name, shape in dbg.items():
        if name.startswith("_"):
            continue
        dbg_outs[name] = dout("dbg_" + name, shape, BF16 if name == "o_tm" else F32)
    out_regs = []

    ident_f = sb("ident_f", [128, 128])
    ident_b = sb("ident_b", [128, 128], BF16)
    R_ident = Reg("ident")
    k.dma("sp", ident_f[:], ident_d[:], writes=[R_ident])
    R_identb = Reg("identb")
    k.op("dve", lambda e: e.tensor_copy(out=ident_b[:], in_=ident_f[:]), reads=[R_ident], writes=[R_identb])

    PS = [ps(f"psb{i}", [128, 512]) for i in range(8)]
    R_PS = [Reg(f"ps{i}") for i in range(8)]
    ps_rr = [0]

    def next_ps():
        i = ps_rr[0]
        ps_rr[0] = (i + 1) % 8
        return PS[i], R_PS[i]

    mod = sb("mod", [128, 96, NCV])
    R_mod = Reg("mod")
    cT = sb("cT_sb", [128, 16 * NCV])
    sT = sb("sT", [128, 16, NCV], BF16)
    R_cT = Reg("cT")
    R_sT = Reg("sT")
    k.dma("sp", cT[:], cT_d[:], writes=[R_cT])
    k.op("act", lambda e: e.activation(out=sT[:].rearrange("p a b -> p (a b)"), in_=cT[:], func=AF.Silu),
         reads=[R_cT], writes=[R_sT])
    bada = sb("bada", [128, 96])
    R_bada = Reg("bada")
    with nc.allow_non_contiguous_dma(reason="small one-time param loads"):
        k.dma("sp", bada[:], b_ada.rearrange("(c p) -> p c", p=128), writes=[R_bada])
        n1 = sb("n1", [128, 16])
        n2 = sb("n2", [128, 16])
        R_n = Reg("n12")
        k.dma("sp", n1[:], norm1_d.rearrange("(c p) -> p c", p=128), writes=[R_n])
        k.dma("sp", n2[:], norm2_d.rearrange("(c p) -> p c", p=128), writes=[R_n])

    WSLOT_BYTES = 16 * 512 * 2
    WB = 256
    wslots = [sb(f"wslot{i}", [128, 16 * WB], BF16) for i in range(2)]
    R_w = [Reg(f"w{i}") for i in range(2)]
    w_rr = [0]

    def next_w():
        i = w_rr[0]
        w_rr[0] = 1 - i
        return wslots[i], R_w[i]

    w_ada_v = w_ada.rearrange("(kc p) f -> p kc f", p=128)
    for fb in range(6 * D // WB):
        wt, rw = next_w()
        wv = wt[:].rearrange("p (kc f) -> p kc f", f=WB)
        k.dma("pool", wv, w_ada_v[:, :, fb * WB:(fb + 1) * WB], writes=[rw])
        pt, rp = next_ps()

        def mm(e, wv=wv, pt=pt):
            ins = None
            for fc in range(WB // 128):
                for kc in range(16):
                    ins = e.matmul(pt[:, fc * NCV:(fc + 1) * NCV], lhsT=wv[:, kc, fc * 128:(fc + 1) * 128],
                                   rhs=sT[:, kc, :], start=(kc == 0), stop=(kc == 15))
            return ins
        k.op("pe", mm, reads=[rw, R_sT], writes=[rp])
        for fc in range(WB // 128):
            idx = fb * (WB // 128) + fc
            k.op("dve", lambda e, idx=idx, fc=fc, pt=pt: e.tensor_scalar(
                out=mod[:, idx, :], in0=pt[:, fc * NCV:(fc + 1) * NCV], scalar1=bada[:, idx:idx + 1], scalar2=None,
                op0=ALU.add), reads=[rp, R_bada], writes=[R_mod])
    g12 = sb("g12", [128, 2, 16, NCV])
    R_g = Reg("g12")
    for j, (nn, s) in enumerate(((n1, 1), (n2, 4))):
        for m in range(16):
            k.op("dve", lambda e, j=j, m=m, nn=nn, s=s: e.tensor_scalar(
                out=g12[:, j, m, :], in0=mod[:, 16 * s + m, :], scalar1=1.0, scalar2=nn[:, m:m + 1],
                op0=ALU.add, op1=ALU.mult), reads=[R_mod, R_n], writes=[R_g])

    if "mod" in dbg:
        R_d = Reg("dbgmod")
        k.dma("sp", dbg_outs["mod"].rearrange("p (a b) -> p a b", b=NCV), mod[:], reads=[R_mod], writes=[R_d])
        out_regs.append(R_d)

    TMAX = max(r[0] for r in reqs)
    hT = sb("hT", [128, 16, TMAX], BF16)
    R_hT = Reg("hT")
    xts = [sb("xt0", [128, D])] * 2
    R_xt = [Reg("xt0")] * 2
    R_junk = Reg("junk")
    stat = sb("stat", [128, 8])
    R_stat = Reg("stat")
    xt_rr = [0]

    def norm_to_T(src_ap_fn, ntiles, gsel, shift_s, cv, dstT, R_dst, R_src_fn=None):
        for tt in range(ntiles):
            i = xt_rr[0]
            xt_rr[0] = 1 - i
            xt, rx = xts[i], R_xt[i]
            k.dma("sp", xt[:], src_ap_fn(tt), reads=([R_src_fn] if R_src_fn else []), writes=[rx])
            k.op("act", lambda e, xt=xt: e.activation(out=junk, in_=xt[:], func=AF.Square, accum_out=stat[:, 0:1]),
                 reads=[rx], writes=[R_junk, R_stat])
            k.op("dve", lambda e: e.tensor_scalar(out=stat[:, 1:2], in0=stat[:, 0:1], scalar1=1.0 / D, scalar2=EPS,
                                                  op0=ALU.mult, op1=ALU.add), reads=[R_stat], writes=[R_stat])
            k.op("act", lambda e: e.activation(out=stat[:, 2:3], in_=stat[:, 1:2], func=AF.Sqrt),
                 reads=[R_stat], writes=[R_stat])
            k.op("dve", lambda e: e.reciprocal(out=stat[:, 3:4], in_=stat[:, 2:3]), reads=[R_stat], writes=[R_stat])
            k.op("dve", lambda e, xt=xt: e.tensor_scalar(out=xt[:], in0=xt[:], scalar1=stat[:, 3:4], scalar2=None,
                                                         op0=ALU.mult), reads=[rx, R_stat], writes=[rx])
            for q4 in range(4):
                pt, rp = next_ps()

                def tr(e, q4=q4, pt=pt, xt=xt):
                    ins = None
                    for j in range(4):
                        m = q4 * 4 + j
                        ins = e.transpose(pt[:, j * 128:(j + 1) * 128], xt[:, m * 128:(m + 1) * 128], ident_f[:])
                    return ins
                k.op("pe", tr, reads=[rx, R_ident], writes=[rp])
                for j in range(4):
                    m = q4 * 4 + j
                    k.op("act", lambda e, m=m, j=j, pt=pt, tt=tt: e.activation(
                        out=dstT[:, m, tt * 128:(tt + 1) * 128], in_=pt[:, j * 128:(j + 1) * 128], func=AF.Identity,
                        scale=g12[:, gsel, m, cv:cv + 1], bias=mod[:, 16 * shift_s + m, cv:cv + 1]),
                        reads=[rp, R_g, R_mod], writes=[R_dst])


    NPR = cfg.get("n_prompt", 2)
    w_in_v = w_in.rearrange("(kc p) f -> p kc f", p=128)
    dlog_d = din("ret_decay_logit", [16])
    gnw_d = din("ret_gn_w", [D])
    cst_d = din("ret_consts", [128, 4 * 128 + 4])
    rope_d = din("rope_tabs", [128, 2 * 1024])
    psw_d = din("rope_psw", [128, 128])
    sret_d = din("state_ret", [2, NH, DK, DV])
    newret = dout("new_ret", [NPR, 2, NH, DK, DV])
    out_regs_ret = Reg("newret")

    cst = sb("cst", [128, 4 * 128 + 4])
    R_cst = Reg("cst")
    k.dma("sp", cst[:], cst_d[:], writes=[R_cst])
    psw = sb("psw", [128, 128])
    R_psw = Reg("psw")
    k.dma("sp", psw[:], psw_d[:], writes=[R_psw])
    gnw = sb("gnw", [128, D])
    R_gnw = Reg("gnw")
    lg = sb("lg", [128, 16])
    R_lg = Reg("lg")
    with nc.allow_non_contiguous_dma(reason="partition-broadcast param loads"):
        k.dma("sp", gnw[:], gnw_d.partition_broadcast(128), writes=[R_gnw])
        k.dma("sp", lg[:], dlog_d.partition_broadcast(128), writes=[R_lg])
    k.op("act", lambda e: e.activation(out=lg[:], in_=lg[:], func=AF.Exp, scale=-1.0), reads=[R_lg], writes=[R_lg])
    k.op("dve", lambda e: e.tensor_scalar(out=lg[:], in0=lg[:], scalar1=1.0, scalar2=None, op0=ALU.add),
         reads=[R_lg], writes=[R_lg])
    k.op("act", lambda e: e.activation(out=lg[:], in_=lg[:], func=AF.Ln), reads=[R_lg], writes=[R_lg])
    k.op("dve", lambda e: e.tensor_scalar(out=lg[:], in0=lg[:], scalar1=-1.0, scalar2=None, op0=ALU.mult),
         reads=[R_lg], writes=[R_lg])
    dec = sb("dec", [128, 3, 16])
    R_dec = Reg("dec")
    for (row, col, sl) in ((0, 512, slice(0, 8)), (0, 513, slice(8, 16)), (1, 514, slice(0, 8)), (1, 515, slice(8, 16))):
        k.op("act", lambda e, row=row, col=col, sl=sl: e.activation(out=dec[:, row, sl], in_=lg[:, sl], func=AF.Exp,
                                                                    scale=cst[:, col:col + 1]),
             reads=[R_lg, R_cst], writes=[R_dec])
    k.op("act", lambda e: e.activation(out=dec[:, 2, :], in_=lg[:], func=AF.Exp, scale=128.0), reads=[R_lg], writes=[R_dec])
    MT = sb("MT", [128, NH, 128])
    mtmp = sb("mtmp", [128, 128])
    R_MT = Reg("MT")
    R_mtmp = Reg("mtmp")
    for h in range(NH):
        k.op("act", lambda e, h=h: e.activation(out=MT[:, h, :], in_=cst[:, 0:128], func=AF.Exp, scale=lg[:, h:h + 1]),
             reads=[R_lg, R_cst], writes=[R_MT])
        k.op("dve", lambda e, h=h: e.tensor_tensor(out=MT[:, h, :], in0=MT[:, h, :], in1=cst[:, 256:384], op=ALU.mult),
             reads=[R_cst], writes=[R_MT])
        k.op("act", lambda e, h=h: e.activation(out=mtmp[:], in_=cst[:, 128:256], func=AF.Exp, scale=lg[:, 8 + h:9 + h]),
             reads=[R_lg, R_cst], writes=[R_mtmp])
        k.op("dve", lambda e, h=h: e.tensor_tensor(out=mtmp[:], in0=mtmp[:], in1=cst[:, 384:512], op=ALU.mult),
             reads=[R_cst], writes=[R_mtmp])
        k.op("dve", lambda e, h=h: e.tensor_tensor(out=MT[:, h, :], in0=MT[:, h, :], in1=mtmp[:], op=ALU.add),
             reads=[R_mtmp], writes=[R_MT])

    def linear_fm(wview, KC, col0, ncols, rhsT, R_rhs, T, epi):
        for cb in range(ncols // WB):
            wt, rw = next_w()
            wv = wt[:, 0:KC * WB].rearrange("p (kc f) -> p kc f", f=WB)
            k.dma("pool", wv, wview[:, :, col0 + cb * WB: col0 + (cb + 1) * WB], writes=[rw])
            for c4 in range(WB // 128):
                for tb in range((T + 511) // 512):
                    N = min(512, T - tb * 512)
                    pt, rp = next_ps()

                    def mm(e, wv=wv, pt=pt, c4=c4, tb=tb, N=N):
                        ins = None
                        for kc in range(KC):
                            ins = e.matmul(pt[:, 0:N], lhsT=wv[:, kc, c4 * 128:(c4 + 1) * 128],
                                           rhs=rhsT[:, kc, tb * 512: tb * 512 + N], start=(kc == 0), stop=(kc == KC - 1))
                        return ins
                    k.op("pe", mm, reads=[rw, R_rhs], writes=[rp])
                    epi(pt, rp, cb * (WB // 128) + c4, tb, N)

    def linear_tm(wview, KC, col0, ncols, lhsT, R_lhs, T, epi):
        for cb in range(ncols // WB):
            wt, rw = next_w()
            wv = wt[:, 0:KC * WB].rearrange("p (kc f) -> p kc f", f=WB)
            k.dma("pool", wv, wview[:, :, col0 + cb * WB: col0 + (cb + 1) * WB], writes=[rw])
            for tt in range(T // 128):
                pt, rp = next_ps()

                def mm(e, wv=wv, pt=pt, tt=tt):
                    ins = None
                    for kc in range(KC):
                        ins = e.matmul(pt[:, 0:WB], lhsT=lhsT[:, kc, tt * 128:(tt + 1) * 128], rhs=wv[:, kc, :],
                                       start=(kc == 0), stop=(kc == KC - 1))
                    return ins
                k.op("pe", mm, reads=[rw, R_lhs], writes=[rp])
                epi(pt, rp, cb, tt)

    NTM = TMAX // 128
    arena = sb("arena", [128, 24576])

    def carve(off_bytes, shape, dt):
        n = 1
        for d_ in shape[1:]:
            n *= d_
        nbytes = n * (2 if dt == BF16 else 4)
        v = arena[:, off_bytes // 4:(off_bytes + nbytes) // 4]
        if dt != F32:
            v = v.bitcast(dt)
        if len(shape) == 3:
            v = v.rearrange("p (a b) -> p a b", b=shape[2])
        return v
    junk = carve(90112, [128, D], BF16)
    qT = carve(0, [128, NH, 1024], BF16)
    kT = carve(16384, [128, NH, 1024], BF16)
    R_qT = Reg("qT")
    R_kT = Reg("kT")
    v_tm = carve(32768, [128, 8, D], BF16)
    R_v = Reg("v_tm")
    o_tm = carve(65536, [128, 8, D], BF16)
    ropet = carve(65536, [128, 2, 1024], F32)
    R_o = Reg("o_tm")
    rtmp = xts[0][:, 0:512]
    R_rtmp = R_xt[0]
    rt1 = xts[0][:, 512:1024]
    R_rt1 = R_xt[0]
    Sst = sb("Sst", [128, DV])
    R_S = Reg("Sst")
    Sfb = sb("Sfb", [128, NTM, DV], BF16)
    Sbb = sb("Sbb", [128, NTM, DV], BF16)
    R_Sfb = Reg("Sfb")
    R_Sbb = Reg("Sbb")
    kd = sb("kd", [128, 128], BF16)
    R_kd = Reg("kd")
    Amat = sb("Amat", [128, 128], BF16)
    R_A = Reg("Amat")
    ot = sb("ot", [128, DV])
    ot2 = sb("ot2", [128, DV])
    R_ot = Reg("ot")
    R_ot2 = Reg("ot2")
    gst = sb("gst", [128, 8])
    R_gst = Reg("gst")
    sg = sb("sg", [128, 512], BF16)
    R_sg = Reg("sg")

    a_re_d = din("s5_a_re", [2, NG, NP_])
    a_im_d = din("s5_a_im", [2, NG, NP_])
    ldt_d = din("s5_log_dt", [2, NG])
    b_re_d = din("s5_b_re", [2, NG, NP_, 16])
    b_im_d = din("s5_b_im", [2, NG, NP_, 16])
    c_re_d = din("s5_c_re", [2, NG, 16, NP_])
    c_im_d = din("s5_c_im", [2, NG, 16, NP_])
    s5d_d = din("s5_d", [S5W])
    rowmask_d = din("s5_rowmask", [128, 4])
    h0re_d = din("state_s5_re", [2, NG, NP_])
    h0im_d = din("state_s5_im", [2, NG, NP_])
    news5 = [dout("new_s5_re", [NPR, 2, NG, NP_]), dout("new_s5_im", [NPR, 2, NG, NP_])]
    R_news5 = Reg("news5")
    btp_scr = nc.dram_tensor("btp_scr", [128, 32 * 128], BF16).ap()
    ctp_scr = nc.dram_tensor("ctp_scr", [128, 128 * 32], BF16).ap()
    R_scr = Reg("s5scr")
    TB = 32
    AAt = sb("AAt", [128, 2, 64])
    BBt = sb("BBt", [128, 2, 64])
    R_AB = Reg("AABB")
    dsk = sb("dsk", [128, 8])
    R_dsk = Reg("dsk")
    negpi = sb("negpi", [128, 1])
    R_negpi = Reg("negpi")
    rowmask = sb("rowmask", [128, 4])
    R_rowmask = Reg("rowmask")
    Xst = sb("Xst", [128, 2, 128])
    R_X = Reg("Xst")
    st1 = sb("st1", [128, 128])
    st2 = sb("st2", [128, 128])
    R_st1 = Reg("st1")
    R_st2 = Reg("st2")
    gyT = sb("gyT", [128, 8, TMAX], BF16)
    R_gyT = Reg("gyT")
    k.op("dve", lambda e: e.memset(negpi[:], -float(np.pi * (1 - 2e-6))), writes=[R_negpi])
    k.dma("sp", rowmask[:], rowmask_d[:], writes=[R_rowmask])
    with nc.allow_non_contiguous_dma(reason="small one-time param loads"):
        k.dma("sp", dsk[:], s5d_d.rearrange("(G p) -> p G", p=128), writes=[R_dsk])

    def s5_precompute():
        R_t = Reg("s5tmp")
        rows = {}
        names = ["ar", "ai", "lr", "li", "er", "ts", "tf", "fr", "sn", "cs", "Ar", "Ai", "nr", "den", "qr", "qi", "w1", "w2"]
        for n_i, nm in enumerate(names):
            rows[nm] = carve(n_i * 512, [64, 128], F32)[0:64, :]
        ti = carve(18 * 512, [64, 128], F32)[0:64, :].bitcast(mybir.dt.int32)
        ldt = carve(19 * 512, [64, 2], F32)[0:64, :]
        Pq = carve(10240, [128, 4, 64], F32)
        Bl = [carve(12288, [128, 64, 16], F32), carve(16384, [128, 64, 16], F32)]
        Bb = [carve(20480, [128, 64, 16], F32), carve(24576, [128, 64, 16], F32)]
        Bt = [carve(28672, [128, 64, 16], F32), carve(32768, [128, 64, 16], F32)]
        BQ = [carve(36864, [128, 16, 128], F32), carve(45056, [128, 16, 128], F32)]
        Cin = [carve(53248, [128, 16, 128], F32), carve(61440, [128, 16, 128], F32)]
        BTp_sb = carve(69632, [128, 32, 128], BF16)
        CTp_sb = carve(77824, [128, 128, 32], BF16)
        k.dma("sp", rows["ar"], a_re_d.rearrange("d (G r) p -> (d G) (r p)", r=2), writes=[R_t])
        k.dma("sp", rows["ai"], a_im_d.rearrange("d (G r) p -> (d G) (r p)", r=2), writes=[R_t])
        k.dma("sp", ldt, ldt_d.rearrange("d (G r) -> (d G) r", r=2), writes=[R_t])

        def T1(eng, fn):
            k.op(eng, fn, reads=[R_negpi], writes=[R_t])
        T1("act", lambda e: e.activation(out=ldt, in_=ldt, func=AF.Exp))
        for par in range(2):
            sl = slice(par * 64, (par + 1) * 64)
            T1("dve", lambda e, sl=sl, par=par: e.tensor_scalar(out=rows["lr"][:, sl], in0=rows["ar"][:, sl],
                                                              scalar1=ldt[:, par:par + 1], scalar2=None, op0=ALU.mult))
            T1("dve", lambda e, sl=sl, par=par: e.tensor_scalar(out=rows["li"][:, sl], in0=rows["ai"][:, sl],
                                                              scalar1=ldt[:, par:par + 1], scalar2=None, op0=ALU.mult))
        T1("act", lambda e: e.activation(out=rows["er"], in_=rows["lr"], func=AF.Exp))
        for (shift, dst) in ((0.5, "sn"), (0.75, "cs")):
            T1("dve", lambda e, shift=shift: e.tensor_scalar(out=rows["ts"], in0=rows["li"], scalar1=1.0 / (2 * np.pi),
                                                            scalar2=shift, op0=ALU.mult, op1=ALU.add))
            T1("dve", lambda e: e.tensor_copy(out=ti, in_=rows["ts"]))
            T1("dve", lambda e: e.tensor_copy(out=rows["tf"], in_=ti))
            T1("dve", lambda e: e.tensor_tensor(out=rows["w1"], in0=rows["tf"], in1=rows["ts"], op=ALU.is_gt))
            T1("dve", lambda e: e.tensor_tensor(out=rows["tf"], in0=rows["tf"], in1=rows["w1"], op=ALU.subtract))
            T1("dve", lambda e: e.tensor_tensor(out=rows["fr"], in0=rows["ts"], in1=rows["tf"], op=ALU.subtract))
            T1("act", lambda e, dst=dst: e.activation(out=rows[dst], in_=rows["fr"], func=AF.Sin, scale=float(2 * np.pi * (1 - 2e-6)),
                                                     bias=negpi[0:64, :]))
        TT = lambda o, a, b, op: T1("dve", lambda e: e.tensor_tensor(out=rows[o], in0=rows[a], in1=rows[b], op=op))
        TT("Ar", "er", "cs", ALU.mult)
        TT("Ai", "er", "sn", ALU.mult)
        T1("dve", lambda e: e.tensor_scalar(out=rows["nr"], in0=rows["Ar"], scalar1=-1.0, scalar2=None, op0=ALU.add))
        TT("w1", "ar", "ar", ALU.mult)
        TT("w2", "ai", "ai", ALU.mult)
        TT("den", "w1", "w2", ALU.add)
        T1("dve", lambda e: e.reciprocal(out=rows["den"], in_=rows["den"]))
        TT("w1", "nr", "ar", ALU.mult)
        TT("w2", "Ai", "ai", ALU.mult)
        TT("w1", "w1", "w2", ALU.add)
        TT("qr", "w1", "den", ALU.mult)
        TT("w1", "Ai", "ar", ALU.mult)
        TT("w2", "nr", "ai", ALU.mult)
        TT("w1", "w1", "w2", ALU.subtract)
        TT("qi", "w1", "den", ALU.mult)
        pt, rp = next_ps()

        def trq(e):
            ins = None
            for j, nm in enumerate(("Ar", "Ai", "qr", "qi")):
                ins = e.transpose(pt[:, j * 64:(j + 1) * 64], rows[nm], ident_f[0:64, 0:64])
            return ins
        k.op("pe", trq, reads=[R_t, R_ident], writes=[rp])
        k.op("dve", lambda e: e.tensor_copy(out=Pq.rearrange("p a b -> p (a b)"), in_=pt[:, 0:256]), reads=[rp], writes=[R_t])
        k.op("dve", lambda e: e.tensor_copy(out=AAt[:, 0, :], in_=Pq[:, 0, :]), reads=[R_t], writes=[R_AB])
        k.op("dve", lambda e: e.tensor_copy(out=AAt[:, 1, :], in_=Pq[:, 0, :]), reads=[R_t], writes=[R_AB])
        k.op("dve", lambda e: e.tensor_copy(out=BBt[:, 1, :], in_=Pq[:, 1, :]), reads=[R_t], writes=[R_AB])
        k.op("dve", lambda e: e.tensor_scalar(out=BBt[:, 0, :], in0=Pq[:, 1, :], scalar1=-1.0, scalar2=None, op0=ALU.mult),
             reads=[R_t], writes=[R_AB])
        with nc.allow_non_contiguous_dma(reason="64B-run param loads"):
            for ri_, bd in enumerate((b_re_d, b_im_d)):
                bv = bd.rearrange("d (G r) p h -> r p (d G) h", r=2)
                for par in range(2):
                    k.dma("sp", Bl[ri_][par * 64:(par + 1) * 64, :, :], bv[par], writes=[R_t])
            for ri_, cd in enumerate((c_re_d, c_im_d)):
                cvw = cd.rearrange("d (G g) h p -> (g h) (d G) p", g=8)
                C4 = Cin[ri_].rearrange("q a (r p) -> q a r p", r=2)
                for dup in range(2):
                    k.dma("sp", C4[:, :, dup, :], cvw, writes=[R_t])
        qrb = Pq[:, 2, :][:, :, None].to_broadcast([128, 64, 16])
        qib = Pq[:, 3, :][:, :, None].to_broadcast([128, 64, 16])
        T1("dve", lambda e: e.tensor_tensor(out=Bt[0], in0=Bl[0], in1=qrb, op=ALU.mult))
        T1("dve", lambda e: e.tensor_tensor(out=Bt[1], in0=Bl[1], in1=qib, op=ALU.mult))
        T1("dve", lambda e: e.tensor_tensor(out=Bb[0], in0=Bt[0], in1=Bt[1], op=ALU.subtract))
        T1("dve", lambda e: e.tensor_tensor(out=Bt[0], in0=Bl[1], in1=qrb, op=ALU.mult))
        T1("dve", lambda e: e.tensor_tensor(out=Bt[1], in0=Bl[0], in1=qib, op=ALU.mult))
        T1("dve", lambda e: e.tensor_tensor(out=Bb[1], in0=Bt[0], in1=Bt[1], op=ALU.add))
        for ri_ in range(2):
            T1("dve", lambda e, ri_=ri_: e.memset(BQ[ri_], 0.0))
            BQ5 = BQ[ri_].rearrange("q a (g r h) -> q (a g) r h", r=2, h=16)
            for par in range(2):
                ps_ = slice(par * 64, (par + 1) * 64)
                T1("dve", lambda e, BQ5=BQ5, par=par, ps_=ps_, ri_=ri_: e.tensor_copy(out=BQ5[ps_, :, par, :], in_=Bb[ri_][ps_, :, :]))
        for ri_ in range(2):
            C4 = Cin[ri_].rearrange("q a (r p) -> q a r p", r=2)
            for par in range(2):
                k.op("dve", lambda e, C4=C4, par=par, ri_=ri_: e.tensor_scalar(
                    out=C4[:, :, par, :], in0=C4[:, :, par, :], scalar1=rowmask[:, 2 * ri_ + par:2 * ri_ + par + 1],
                    scalar2=None, op0=ALU.mult), reads=[R_rowmask], writes=[R_t])
        CT6 = CTp_sb.rearrange("q (a g r) m -> q a g r m", g=4, r=2)
        for a in range(16):
            pt, rp = next_ps()

            def trb(e, a=a, pt=pt):
                ins = None
                for ri_ in range(2):
                    ins = e.transpose(pt[:, ri_ * 128:(ri_ + 1) * 128], BQ[ri_][:, a, :], ident_f[:])
                    ins = e.transpose(pt[:, 256 + ri_ * 128:256 + (ri_ + 1) * 128], Cin[ri_][:, a, :], ident_f[:])
                return ins
            k.op("pe", trb, reads=[R_t, R_ident], writes=[rp])
            k.op("act", lambda e, a=a, pt=pt: e.activation(out=BTp_sb[:, 2 * a:2 * a + 2, :].rearrange("p a b -> p (a b)"),
                                                         in_=pt[:, 0:256], func=AF.Copy), reads=[rp], writes=[R_t])
            for ri_ in range(2):
                k.op("dve", lambda e, a=a, pt=pt, ri_=ri_: e.tensor_copy(
                    out=CT6[:, a, :, ri_, :], in_=pt[:, 256 + ri_ * 128:256 + (ri_ + 1) * 128].rearrange("p (g m) -> p g m", m=32)),
                    reads=[rp], writes=[R_t])
        k.dma("sp", btp_scr, BTp_sb.rearrange("p a b -> p (a b)"), reads=[R_t], writes=[R_scr])
        k.dma("sp", ctp_scr, CTp_sb.rearrange("p a b -> p (a b)"), reads=[R_t], writes=[R_scr])
        k.barrier()

    def s5_branch(T, latent, pidx):
        k.barrier()
        uT = carve(0, [128, 8, 1024], BF16)
        BTp = carve(16384, [128, 32, 128], BF16)
        CTp = carve(24576, [128, 128, 32], BF16)
        bu = carve(32768, [128, 128, TB], F32)
        hist = carve(49152, [128, 128, TB], BF16)
        yacc = carve(65536, [128, 8, 1024], F32)
        R_uT, R_BT, R_CT, R_bu, R_hist, R_yacc = (Reg(n) for n in ("uT", "BTp", "CTp", "bu", "hist", "yacc"))
        if not cfg.get("s5_noscr"):
            k.dma("sp", BTp.rearrange("p a b -> p (a b)"), btp_scr, reads=[R_scr], writes=[R_BT])
            k.dma("sp", CTp.rearrange("p a b -> p (a b)"), ctp_scr, reads=[R_scr], writes=[R_CT])

        def epi_u(pt, rp, cc, tb, N):
            tsl = slice(tb * 512, tb * 512 + N)
            k.op("act", lambda e: e.activation(out=uT[:, cc, tsl], in_=pt[:, 0:N], func=AF.Copy), reads=[rp], writes=[R_uT])
            k.op("dve", lambda e: e.tensor_scalar(out=yacc[:, cc, tsl], in0=pt[:, 0:N], scalar1=dsk[:, cc:cc + 1], scalar2=None,
                                                  op0=ALU.mult), reads=[rp, R_dsk, R_uT], writes=[R_yacc])
        if not cfg.get("s5_nou"):
            linear_fm(w_in_v, 16, C_U, 1024, hT, R_hT, T, epi_u)
        if latent:
            R_h0 = Reg("h0rows")
            h0rows = carve(57344, [64, 2, 128], F32)[0:64, :, :]
            k.dma("sp", h0rows[:, 0, :], h0re_d.rearrange("d (G r) p -> (d G) (r p)", r=2), writes=[R_h0])
            k.dma("sp", h0rows[:, 1, :], h0im_d.rearrange("d (G r) p -> (d G) (r p)", r=2), writes=[R_h0])
            pt, rp = next_ps()

            def trh(e, pt=pt):
                e.transpose(pt[:, 0:64], h0rows[:, 0, :], ident_f[0:64, 0:64])
                return e.transpose(pt[:, 64:128], h0rows[:, 1, :], ident_f[0:64, 0:64])
            k.op("pe", trh, reads=[R_h0, R_ident], writes=[rp])
            k.op("dve", lambda e, pt=pt: e.tensor_copy(out=Xst[:, 0, :], in_=pt[:, 0:128]), reads=[rp], writes=[R_X])
        else:
            k.op("dve", lambda e: e.memset(Xst[:, 0, :], 0.0), writes=[R_X])
        stage = cfg.get("s5_stage", 9)
        if stage <= 1:
            k.barrier()
            return
        bu6 = bu.rearrange("q (r d g w) t -> q r d g w t", r=2, d=2, g=8, w=4)
        AAf = AAt[:].rearrange("p a b -> p (a b)")
        BBf = BBt[:].rearrange("p a b -> p (a b)")
        pp = 0
        nb = T // TB
        for kb in range(nb):
            f0 = kb * TB
            b0 = T - (kb + 1) * TB
            for di in range(2):
                banks = [next_ps() for _ in range(4)]

                def mmbu(e, di=di, banks=banks, f0=f0, b0=b0):
                    ins = None
                    for ri_ in range(2):
                        for G in range(8):
                            j = ri_ * 8 + G
                            for rg in range(cfg.get('s5_nrg', 4)):
                                rs = slice(rg * 32, (rg + 1) * 32)
                                if di == 0:
                                    rhs = uT[rs, G, f0:f0 + TB]
                                else:
                                    rhs = uT[rs, G, b0:b0 + TB][:, ::-1]
                                ins = e.matmul(banks[rg][0][:, j * TB:(j + 1) * TB], lhsT=BTp[rs, (di * 8 + G) * 2 + ri_, :],
                                               rhs=rhs, start=True, stop=True, tile_position=(rg * 32, 0))
                    return ins
                k.op("pe", mmbu, reads=[R_uT, R_BT], writes=[b[1] for b in banks])
                for rg in range(4):
                    eng = "act" if rg % 2 else "dve"
                    src = banks[rg][0][:, 0:16 * TB].rearrange("p (r g t) -> p r g t", r=2, t=TB)
                    dst = bu6[:, :, di, :, rg, :]
                    if eng == "act":
                        k.op("act", lambda e, src=src, dst=dst: e.activation(out=dst, in_=src, func=AF.Copy),
                             reads=[banks[rg][1]], writes=[R_bu])
                    else:
                        k.op("dve", lambda e, src=src, dst=dst: e.tensor_copy(out=dst, in_=src),
                             reads=[banks[rg][1]], writes=[R_bu])
            if stage <= 2:
                continue
            for s_ in range(TB):
                Xc = Xst[:, pp, :]
                Xn = Xst[:, 1 - pp, :]
                Xsw = Xc.rearrange("p (r m) -> p r m", r=2)[:, ::-1, :]
                k.op("dve", lambda e, Xc=Xc: e.tensor_tensor(out=st1[:], in0=Xc, in1=AAf, op=ALU.mult),
                     reads=[R_X, R_AB], writes=[R_st1])
                k.op("dve", lambda e, Xsw=Xsw: e.tensor_tensor(out=st2[:].rearrange("p (r m) -> p r m", r=2), in0=Xsw,
                                                             in1=BBt[:], op=ALU.mult), reads=[R_X, R_AB], writes=[R_st2])
                k.op("dve", lambda e: e.tensor_tensor(out=st1[:], in0=st1[:], in1=st2[:], op=ALU.add),
                     reads=[R_st2], writes=[R_st1])
                k.op("dve", lambda e, Xn=Xn, s_=s_: e.tensor_tensor(out=Xn, in0=st1[:], in1=bu[:, :, s_], op=ALU.add),
                     reads=[R_st1, R_bu], writes=[R_X])
                k.op("act", lambda e, Xn=Xn, s_=s_: e.activation(out=hist[:, :, s_], in_=Xn, func=AF.Copy),
                     reads=[R_X], writes=[R_hist])
                pp = 1 - pp
            if stage <= 3:
                continue
            pty, rpy = next_ps()

            def mmy(e, pty=pty):
                ins = None
                for di in range(2):
                    for G in range(8):
                        for j in range(4):
                            G2 = 4 * G + j
                            for ri_ in range(2):
                                ins = e.matmul(pty[32 * j:32 * j + 32, (di * 8 + G) * TB:(di * 8 + G + 1) * TB],
                                               lhsT=CTp[:, (di * 32 + G2) * 2 + ri_, :], rhs=hist[:, ri_ * 64 + di * 32 + G2, :],
                                               start=(ri_ == 0), stop=(ri_ == 1), tile_position=(0, 32 * j))
                return ins
            k.op("pe", mmy, reads=[R_hist, R_CT], writes=[rpy])
            k.op("dve", lambda e, pty=pty, f0=f0: e.tensor_tensor(
                out=yacc[:, :, f0:f0 + TB], in0=yacc[:, :, f0:f0 + TB],
                in1=pty[:, 0:8 * TB].rearrange("p (g t) -> p g t", t=TB), op=ALU.add), reads=[rpy], writes=[R_yacc])
            k.op("dve", lambda e, pty=pty, b0=b0: e.tensor_tensor(
                out=yacc[:, :, b0:b0 + TB], in0=yacc[:, :, b0:b0 + TB],
                in1=pty[:, 8 * TB:16 * TB].rearrange("p (g t) -> p g t", t=TB)[:, :, ::-1], op=ALU.add),
                reads=[rpy], writes=[R_yacc])
        if not latent:
            pt, rp = next_ps()
            Xf = Xst[:, pp, :]
            k.op("pe", lambda e, pt=pt, Xf=Xf: (e.transpose(pt[0:64, 0:128], Xf[:, 0:64], ident_f[:]),
                                              e.transpose(pt[0:64, 128:256], Xf[:, 64:128], ident_f[:]))[1],
                 reads=[R_X, R_ident], writes=[rp])
            fin = carve(57344, [64, 256], F32)[0:64, :]
            R_fin = Reg("s5fin")
            k.op("dve", lambda e, pt=pt: e.tensor_copy(out=fin, in_=pt[0:64, 0:256]), reads=[rp], writes=[R_fin])
            for ri_ in range(2):
                k.dma("sp", news5[ri_][pidx].rearrange("d (G r) p -> (d G) (r p)", r=2), fin[:, ri_ * 128:(ri_ + 1) * 128],
                      reads=[R_fin], writes=[R_news5])
        if "s5y" in dbg and dbg_cur[0]:
            R_d = Reg("dbgs5y")
            k.dma("sp", dbg_outs["s5y"].rearrange("p (a b) -> p a b", b=T), yacc[:, :, 0:T], reads=[R_yacc], writes=[R_d])
            out_regs.append(R_d)
        gw = carve(0, [128, 8, 1024], F32)
        R_gw = Reg("gw")
        ya = yacc[:, :, 0:T]
        gv = gw[:, :, 0:T]
        k.op("dve", lambda e: e.tensor_tensor(out=gv, in0=ya, in1=ya, op=ALU.mult), reads=[R_yacc, R_uT, R_BT, R_CT, R_hist, R_bu], writes=[R_gw])
        k.op("dve", lambda e: e.tensor_scalar(out=gv, in0=gv, scalar1=0.044715, scalar2=1.0, op0=ALU.mult, op1=ALU.add),
             reads=[R_gw], writes=[R_gw])
        k.op("dve", lambda e: e.tensor_tensor(out=gv, in0=gv, in1=ya, op=ALU.mult), reads=[R_yacc], writes=[R_gw])
        k.op("act", lambda e: e.activation(out=gv, in_=gv, func=AF.Sigmoid, scale=1.5957691216), reads=[R_gw], writes=[R_gw])
        k.op("dve", lambda e: e.tensor_tensor(out=gyT[:, :, 0:T], in0=gv, in1=ya, op=ALU.mult), reads=[R_gw, R_yacc], writes=[R_gyT])
        k.barrier()

    def retention(T, latent, off, pidx):
        nt = T // 128
        R_rope = R_o
        if latent:
            k.dma("sp", ropet.rearrange("p a b -> p (a b)"), rope_d[:], writes=[R_o])

        def epi_qk(dst, R_dst, scale):
            def epi(pt, rp, cc, tb, N):
                h = cc
                tsl = slice(tb * 512, tb * 512 + N)
                if not latent:
                    k.op("act", lambda e: e.activation(out=dst[:, h, tsl], in_=pt[:, 0:N], func=AF.Copy, scale=scale),
                         reads=[rp], writes=[R_dst])
                    return
                k.op("act", lambda e: e.activation(out=rtmp[:, 0:N], in_=pt[:, 0:N], func=AF.Copy, scale=scale),
                     reads=[rp], writes=[R_rtmp])
                p2, rp2 = next_ps()
                k.op("pe", lambda e: e.matmul(p2[:, 0:N], lhsT=psw[:], rhs=rtmp[:, 0:N], start=True, stop=True),
                     reads=[R_rtmp, R_psw], writes=[rp2])
                k.op("dve", lambda e: e.tensor_tensor(out=rt1[:, 0:N], in0=p2[:, 0:N], in1=ropet[:, 1, tsl], op=ALU.mult),
                     reads=[rp2, R_rope], writes=[R_rt1])
                k.op("dve", lambda e: e.tensor_tensor(out=rtmp[:, 0:N], in0=rtmp[:, 0:N], in1=ropet[:, 0, tsl], op=ALU.mult),
                     reads=[R_rope], writes=[R_rtmp])
                k.op("dve", lambda e: e.tensor_tensor(out=dst[:, h, tsl], in0=rtmp[:, 0:N], in1=rt1[:, 0:N], op=ALU.add),
                     reads=[R_rtmp, R_rt1], writes=[R_dst])
            return epi
        linear_fm(w_in_v, 16, C_Q, 1024, hT, R_hT, T, epi_qk(qT, R_qT, DK ** -0.5))
        linear_fm(w_in_v, 16, C_K, 1024, hT, R_hT, T, epi_qk(kT, R_kT, 1.0))

        def epi_v(pt, rp, cb, tt):
            k.op("act", lambda e: e.activation(out=v_tm[:, tt, cb * WB:(cb + 1) * WB], in_=pt[:, 0:WB], func=AF.Copy),
                 reads=[rp], writes=[R_v])
        linear_tm(w_in_v, 16, C_V, 2048, hT, R_hT, T, epi_v)

        for h in cfg.get('ret_head_list', list(range(cfg.get('ret_heads', NH)))):
            for di, (Sb_, R_Sb, order) in enumerate(((Sfb, R_Sfb, list(range(nt))), (Sbb, R_Sbb, list(range(nt - 1, -1, -1))))):
                col = di * 8 + h
                if latent:
                    k.dma("sp", Sst[:], sret_d[di, h, :, :], writes=[R_S])
                else:
                    k.op("dve", lambda e: e.memset(Sst[:], 0.0), writes=[R_S])
                for c in order:
                    k.op("act", lambda e, c=c, Sb_=Sb_: e.activation(out=Sb_[:, c, :], in_=Sst[:], func=AF.Copy),
                         reads=[R_S], writes=[R_Sb])
                    pt, rp = next_ps()
                    k.op("pe", lambda e, c=c, pt=pt: e.matmul(pt[:, 0:128], lhsT=kT[:, h, c * 128:(c + 1) * 128],
                                                             rhs=ident_b[:], start=True, stop=True),
                         reads=[R_kT, R_identb], writes=[rp])
                    k.op("dve", lambda e, pt=pt, col=col: e.tensor_scalar(out=kd[:], in0=pt[:, 0:128],
                                                                         scalar1=dec[:, 1, col:col + 1], scalar2=None,
                                                                         op0=ALU.mult), reads=[rp, R_dec], writes=[R_kd])
                    p2, rp2 = next_ps()
                    k.op("pe", lambda e, c=c, p2=p2: e.matmul(p2[:, 0:DV], lhsT=kd[:], rhs=v_tm[:, c, h * DV:(h + 1) * DV],
                                                             start=True, stop=True), reads=[R_kd, R_v], writes=[rp2])
                    k.op("dve", lambda e, p2=p2, col=col: e.scalar_tensor_tensor(
                        out=Sst[:], in0=Sst[:], scalar=dec[:, 2, col:col + 1], in1=p2[:, 0:DV], op0=ALU.mult, op1=ALU.add),
                        reads=[rp2, R_dec], writes=[R_S])
                if not latent:
                    k.dma("sp", newret[pidx, di, h, :, :], Sst[:], reads=[R_S], writes=[out_regs_ret])
            for c in range(nt):
                csl = slice(c * 128, (c + 1) * 128)
                pt, rp = next_ps()
                k.op("pe", lambda e, pt=pt, csl=csl: e.matmul(pt[:, 0:128], lhsT=kT[:, h, csl], rhs=qT[:, h, csl],
                                                             start=True, stop=True), reads=[R_kT, R_qT], writes=[rp])
                k.op("dve", lambda e, pt=pt: e.tensor_tensor(out=Amat[:], in0=pt[:, 0:128], in1=MT[:, h, :], op=ALU.mult),
                     reads=[rp, R_MT], writes=[R_A])
                p2, rp2 = next_ps()

                def mm2(e, p2=p2, c=c, csl=csl):
                    e.matmul(p2[:, 0:DV], lhsT=Amat[:], rhs=v_tm[:, c, h * DV:(h + 1) * DV], start=True, stop=True)
                    return e.matmul(p2[:, DV:2 * DV], lhsT=qT[:, h, csl], rhs=Sfb[:, c, :], start=True, stop=True)
                k.op("pe", mm2, reads=[R_A, R_v, R_qT, R_Sfb], writes=[rp2])
                p3, rp3 = next_ps()
                k.op("pe", lambda e, p3=p3, c=c, csl=csl: e.matmul(p3[:, 0:DV], lhsT=qT[:, h, csl], rhs=Sbb[:, c, :],
                                                                  start=True, stop=True), reads=[R_qT, R_Sbb], writes=[rp3])
                k.op("dve", lambda e, p2=p2: e.tensor_scalar(out=ot[:], in0=p2[:, DV:2 * DV], scalar1=dec[:, 0, h:h + 1],
                                                             scalar2=None, op0=ALU.mult), reads=[rp2, R_dec], writes=[R_ot])
                k.op("dve", lambda e, p3=p3: e.scalar_tensor_tensor(out=ot2[:], in0=p3[:, 0:DV], scalar=dec[:, 0, 8 + h:9 + h],
                                                                    in1=ot[:], op0=ALU.mult, op1=ALU.add),
                     reads=[rp3, R_dec, R_ot], writes=[R_ot2])
                k.op("dve", lambda e, p2=p2: e.tensor_tensor(out=ot[:], in0=p2[:, 0:DV], in1=ot2[:], op=ALU.add),
                     reads=[rp2, R_ot2], writes=[R_ot])
                k.op("act", lambda e: e.activation(out=ot2[:], in_=ot[:], func=AF.Identity, accum_out=gst[:, 0:1]),
                     reads=[R_ot], writes=[R_ot2, R_gst])
                k.op("dve", lambda e: e.tensor_scalar(out=gst[:, 1:2], in0=gst[:, 0:1], scalar1=-1.0 / DV, scalar2=None,
                                                      op0=ALU.mult), reads=[R_gst], writes=[R_gst])
                k.op("dve", lambda e: e.tensor_scalar(out=ot[:], in0=ot[:], scalar1=gst[:, 1:2], scalar2=None,
                                                      op0=ALU.add), reads=[R_gst], writes=[R_ot])
                k.op("act", lambda e: e.activation(out=ot2[:], in_=ot[:], func=AF.Square, accum_out=gst[:, 2:3]),
                     reads=[R_ot], writes=[R_ot2, R_gst])
                k.op("dve", lambda e: e.tensor_scalar(out=gst[:, 3:4], in0=gst[:, 2:3], scalar1=1.0 / DV, scalar2=EPS,
                                                      op0=ALU.mult, op1=ALU.add), reads=[R_gst], writes=[R_gst])
                k.op("act", lambda e: e.activation(out=gst[:, 4:5], in_=gst[:, 3:4], func=AF.Sqrt),
                     reads=[R_gst], writes=[R_gst])
                k.op("dve", lambda e: e.reciprocal(out=gst[:, 5:6], in_=gst[:, 4:5]), reads=[R_gst], writes=[R_gst])
                k.op("dve", lambda e, c=c: e.scalar_tensor_tensor(out=o_tm[:, c, h * DV:(h + 1) * DV], in0=ot[:],
                                                                  scalar=gst[:, 5:6], in1=gnw[:, h * DV:(h + 1) * DV],
                                                                  op0=ALU.mult, op1=ALU.mult),
                     reads=[R_ot, R_gst, R_gnw], writes=[R_o])

        def epi_g(pt, rp, cb, tt):
            k.op("act", lambda e: e.activation(out=sg[:, 0:WB], in_=pt[:, 0:WB], func=AF.Silu), reads=[rp], writes=[R_sg])
            k.op("dve", lambda e: e.tensor_tensor(out=o_tm[:, tt, cb * WB:(cb + 1) * WB],
                                                  in0=o_tm[:, tt, cb * WB:(cb + 1) * WB], in1=sg[:, 0:WB], op=ALU.mult),
                 reads=[R_sg], writes=[R_o])
        linear_tm(w_in_v, 16, C_G, 2048, hT, R_hT, T, epi_g)

    w_glu_v = din("w_s5_glu", [S5W, 2 * D]).rearrange("(kc p) f -> p kc f", p=128)
    w_ro_v = din("w_ret_out", [D, D]).rearrange("(kc p) f -> p kc f", p=128)
    w_out_v = din("w_out", [D, D]).rearrange("(kc p) f -> p kc f", p=128)
    w_router_d = din("w_router", [D, NE])
    moec_d = din("moe_consts", [128, 384])
    x2_scr = nc.dram_tensor("x2_scr", [NTOK, D], F32).ap()
    h2_scr = nc.dram_tensor("h2_scr", [NTOK, D], BF16).ap()
    R_x2 = Reg("x2scr")
    R_h2s = Reg("h2scr")
    NGT = NTOK // 128
    aff_all = sb("aff_all", [128, NGT, NE])
    affhl = sb("affhl", [128, NGT, NE, 2], BF16)
    posm_all = sb("posm_all", [128, NGT, NE])
    sel_all = sb("sel_all", [128, NGT, NE])
    R_sel = Reg("sel")
    R_aff = Reg("aff")
    R_posm = Reg("posm")
    moec = sb("moec", [128, 384])
    R_moec = Reg("moec")
    k.dma("sp", moec[:], moec_d[:], writes=[R_moec])
    iota_row = moec[:, 0:128]
    onesUb = sb("onesUb", [128, 2, 128], BF16)
    R_oUb = Reg("onesUb")
    k.op("dve", lambda e: e.tensor_copy(out=onesUb[:].rearrange("p a b -> p (a b)"), in_=moec[:, 128:384]), reads=[R_moec], writes=[R_oUb])
    wr = sb("wr", [128, 16, NE], BF16)
    R_wr = Reg("wr")
    k.dma("pool", wr[:], w_router_d.rearrange("(kc p) e -> p kc e", p=128), writes=[R_wr])
    sm = sb("sm", [128, 8])
    R_sm = Reg("sm")
    selt = sb("selt", [128, NE], BF16)
    R_selt = Reg("selt")

    def lin_chunk(wview, KC, col, rhsT, R_rhs, T, epi):
        wt, rw = next_w()
        wv = wt[:, 0:KC * 128].rearrange("p (kc f) -> p kc f", f=128)
        k.dma("pool", wv, wview[:, :, col:col + 128], writes=[rw])
        for tb in range((T + 511) // 512):
            N = min(512, T - tb * 512)
            pt, rp = next_ps()

            def mm(e, wv=wv, pt=pt, tb=tb, N=N):
                ins = None
                for kc in range(KC):
                    ins = e.matmul(pt[:, 0:N], lhsT=wv[:, kc, :], rhs=rhsT[:, kc, tb * 512: tb * 512 + N],
                                   start=(kc == 0), stop=(kc == KC - 1))
                return ins
            k.op("pe", mm, reads=[rw, R_rhs], writes=[rp])
            epi(pt, rp, slice(tb * 512, tb * 512 + N), N)

    def merge_out(T, cv, off):
        nt = T // 128
        k.barrier()
        oT = carve(0, [128, 16, 1024], BF16)
        mergedT = carve(32768, [128, 16, 1024], BF16)
        R_oT = Reg("oT")
        R_mT = Reg("mergedT")
        for m in range(16):
            for tg in range((nt + 3) // 4):
                tiles = list(range(tg * 4, min(nt, tg * 4 + 4)))
                pt, rp = next_ps()

                def tr(e, m=m, tiles=tiles, pt=pt):
                    ins = None
                    for j, tt in enumerate(tiles):
                        ins = e.matmul(pt[:, j * 128:(j + 1) * 128], lhsT=o_tm[:, tt, m * 128:(m + 1) * 128], rhs=ident_b[:],
                                       start=True, stop=True)
                    return ins
                k.op("pe", tr, reads=[R_o, R_identb], writes=[rp])
                n_ = len(tiles) * 128
                eng = "act" if (m + tg) % 2 else "dve"
                if eng == "act":
                    k.op("act", lambda e, m=m, tg=tg, n_=n_, pt=pt: e.activation(out=oT[:, m, tg * 512: tg * 512 + n_], in_=pt[:, 0:n_],
                                                                               func=AF.Copy), reads=[rp], writes=[R_oT])
                else:
                    k.op("dve", lambda e, m=m, tg=tg, n_=n_, pt=pt: e.tensor_copy(out=oT[:, m, tg * 512: tg * 512 + n_], in_=pt[:, 0:n_]),
                         reads=[rp], writes=[R_oT])
        k.barrier()
        tA = carve(65536, [128, 1024], F32)
        tB = carve(69632, [128, 1024], F32)
        g1row = carve(73728, [128, D], F32)
        xb = [carve(81920 + i_ * 1024, [128, 256], F32) for i_ in range(2)]
        x2b = [carve(83968 + i_ * 1024, [128, 256], F32) for i_ in range(2)]
        gbm = carve(86016, [128, 128], F32)
        R_tA, R_tB, R_g1, R_gbm = Reg("tA"), Reg("tB"), Reg("g1row"), Reg("gbm")
        R_xb = [Reg("xb0"), Reg("xb1")]
        R_x2b = [Reg("x2b0"), Reg("x2b1")]
        for m in range(16):
            def e1(pt, rp, tsl, N):
                k.op("act", lambda e: e.activation(out=tA[:, tsl], in_=pt[:, 0:N], func=AF.Sigmoid), reads=[rp], writes=[R_tA])
            lin_chunk(w_glu_v, 8, D + m * 128, gyT, R_gyT, T, e1)

            def e2(pt, rp, tsl, N):
                k.op("dve", lambda e: e.tensor_tensor(out=tA[:, tsl], in0=tA[:, tsl], in1=pt[:, 0:N], op=ALU.mult), reads=[rp], writes=[R_tA])
            lin_chunk(w_glu_v, 8, m * 128, gyT, R_gyT, T, e2)

            def e3(pt, rp, tsl, N):
                k.op("act", lambda e: e.activation(out=tB[:, tsl], in_=pt[:, 0:N], func=AF.Sigmoid), reads=[rp], writes=[R_tB])
                k.op("dve", lambda e: e.tensor_tensor(out=tA[:, tsl], in0=tA[:, tsl], in1=tB[:, tsl], op=ALU.mult), reads=[R_tB], writes=[R_tA])
            lin_chunk(w_in_v, 16, C_GA + m * 128, hT, R_hT, T, e3)

            def e4(pt, rp, tsl, N):
                k.op("act", lambda e: e.activation(out=tB[:, tsl], in_=pt[:, 0:N], func=AF.Sigmoid), reads=[rp], writes=[R_tB])
            lin_chunk(w_in_v, 16, C_GB + m * 128, hT, R_hT, T, e4)

            def e5(pt, rp, tsl, N, m=m):
                k.op("dve", lambda e: e.tensor_tensor(out=tB[:, tsl], in0=tB[:, tsl], in1=pt[:, 0:N], op=ALU.mult), reads=[rp], writes=[R_tB])
                k.op("dve", lambda e: e.tensor_tensor(out=mergedT[:, m, tsl], in0=tA[:, tsl], in1=tB[:, tsl], op=ALU.add),
                     reads=[R_tA, R_tB], writes=[R_mT])
            lin_chunk(w_ro_v, 16, m * 128, oT, R_oT, T, e5)
        bcast_row(32, cv, g1row, R_g1, gbm, R_gbm)

        def epi_o(pt, rp, cb, tt):
            i_ = (cb + tt) % 2
            cols = slice(cb * WB, (cb + 1) * WB)
            rows = slice(off + tt * 128, off + (tt + 1) * 128)
            k.dma("sp", xb[i_], xin[rows, cols], writes=[R_xb[i_]])
            k.op("dve", lambda e: e.tensor_tensor(out=x2b[i_], in0=pt[:, 0:WB], in1=g1row[:, cols], op=ALU.mult),
                 reads=[rp, R_g1], writes=[R_x2b[i_]])
            k.op("dve", lambda e: e.tensor_tensor(out=x2b[i_], in0=x2b[i_], in1=xb[i_], op=ALU.add), reads=[R_xb[i_]], writes=[R_x2b[i_]])
            k.dma("sp", x2_scr[rows, cols], x2b[i_], reads=[R_x2b[i_]], writes=[R_x2])
        linear_tm(w_out_v, 16, 0, D, mergedT, R_mT, T, epi_o)
        k.barrier()

    def bcast_row(sidx, cv, dst, R_dst, gbm, R_gbm):
        for q4 in range(4):
            pt, rp = next_ps()
            for j in range(4):
                m = q4 * 4 + j
                k.op("dve", lambda e, m=m: e.tensor_copy(out=gbm, in_=mod[:, sidx + m, cv:cv + 1].to_broadcast([128, 128])),
                     reads=[R_mod], writes=[R_gbm])
                k.op("pe", lambda e, j=j, pt=pt: e.matmul(pt[:, j * 128:(j + 1) * 128], lhsT=gbm, rhs=ident_f[:], start=True, stop=True),
                     reads=[R_gbm, R_ident], writes=[rp])
            k.op("act", lambda e, q4=q4, pt=pt: e.activation(out=dst[:, q4 * 512:(q4 + 1) * 512], in_=pt[:, :], func=AF.Copy),
                 reads=[rp], writes=[R_dst])

    def norm2_router(T, cv, off, cap):
        nt = T // 128
        gt0 = off // 128
        norm_to_T(lambda tt: x2_scr[off + tt * 128: off + (tt + 1) * 128, :], nt, 1, 3, cv, hT, R_hT, R_src_fn=R_x2)
        k.barrier()
        h2t = [carve(0 + i_ * 4096, [128, D], BF16) for i_ in range(2)]
        R_h2t = [Reg("h2t0"), Reg("h2t1")]
        affT = carve(8192, [16, 1024], F32)[0:16, :]
        R_affT = Reg("affT")
        bcs = carve(12288, [128, 1024], F32)
        R_bcs = Reg("bcs")
        cmpj = carve(16384, [128, 1024], BF16)
        dj = carve(18432, [128, 128], F32)
        R_cmpj = Reg("cmpj")
        for tt in range(nt):
            gt = gt0 + tt
            i_ = tt % 2
            tsl = slice(tt * 128, (tt + 1) * 128)
            for q4 in range(4):
                pt, rp = next_ps()

                def trb(e, q4=q4, pt=pt, tsl=tsl):
                    ins = None
                    for j in range(4):
                        ins = e.matmul(pt[:, j * 128:(j + 1) * 128], lhsT=hT[:, q4 * 4 + j, tsl], rhs=ident_b[:], start=True, stop=True)
                    return ins
                k.op("pe", trb, reads=[R_hT, R_identb], writes=[rp])
                k.op("act", lambda e, q4=q4, pt=pt, i_=i_: e.activation(out=h2t[i_][:, q4 * 512:(q4 + 1) * 512], in_=pt[:, :], func=AF.Copy),
                     reads=[rp], writes=[R_h2t[i_]])
            k.dma("sp", h2_scr[off + tt * 128: off + (tt + 1) * 128, :], h2t[i_], reads=[R_h2t[i_]], writes=[R_h2s])
            pt, rp = next_ps()

            def mmr(e, pt=pt, tsl=tsl):
                ins = None
                for kc in range(16):
                    ins = e.matmul(pt[:, 0:NE], lhsT=hT[:, kc, tsl], rhs=wr[:, kc, :], start=(kc == 0), stop=(kc == 15))
                return ins
            k.op("pe", mmr, reads=[R_hT, R_wr], writes=[rp])
            k.op("dve", lambda e, pt=pt: e.tensor_reduce(out=sm[:, 0:1], in_=pt[:, 0:NE], op=ALU.max, axis=AX.X), reads=[rp], writes=[R_sm])
            k.op("dve", lambda e: e.tensor_scalar(out=sm[:, 1:2], in0=sm[:, 0:1], scalar1=-1.0, scalar2=None, op0=ALU.mult),
                 reads=[R_sm], writes=[R_sm])
            k.op("act", lambda e, pt=pt, gt=gt: e.activation(out=aff_all[:, gt, :], in_=pt[:, 0:NE], func=AF.Exp, bias=sm[:, 1:2],
                                                            accum_out=sm[:, 2:3]), reads=[rp, R_sm], writes=[R_aff, R_sm])
            k.op("dve", lambda e: e.reciprocal(out=sm[:, 3:4], in_=sm[:, 2:3]), reads=[R_sm], writes=[R_sm])
            k.op("dve", lambda e, gt=gt: e.tensor_scalar(out=aff_all[:, gt, :], in0=aff_all[:, gt, :], scalar1=sm[:, 3:4], scalar2=None,
                                                         op0=ALU.mult), reads=[R_sm], writes=[R_aff])
            k.op("dve", lambda e, gt=gt: e.tensor_copy(out=affhl[:, gt, :, 0], in_=aff_all[:, gt, :]), reads=[R_aff], writes=[R_aff])
            k.op("dve", lambda e, gt=gt: e.tensor_tensor(out=affhl[:, gt, :, 1], in0=aff_all[:, gt, :], in1=affhl[:, gt, :, 0],
                                                         op=ALU.subtract), reads=[R_aff], writes=[R_aff])
            pt2, rp2 = next_ps()
            k.op("pe", lambda e, pt2=pt2, gt=gt: e.transpose(pt2[0:16, 0:128], aff_all[:, gt, :], ident_f[:]),
                 reads=[R_aff, R_ident], writes=[rp2])
            k.op("dve", lambda e, pt2=pt2, tsl=tsl: e.tensor_copy(out=affT[:, tsl], in_=pt2[0:16, 0:128]), reads=[rp2], writes=[R_affT])
        oh = carve(20992, [128, 128], F32)[0:16, :]
        R_oh = Reg("oh")
        for ex in range(NE):
            k.op("dve", lambda e, ex=ex: e.tensor_copy(out=oh, in_=ident_f[0:16, ex:ex + 1].to_broadcast([16, 128])),
                 reads=[R_ident], writes=[R_oh])
            for tb in range((T + 511) // 512):
                N = min(512, T - tb * 512)
                pt, rp = next_ps()
                k.op("pe", lambda e, pt=pt, tb=tb, N=N, ex=ex: e.matmul(pt[:, 0:N], lhsT=oh, rhs=affT[:, tb * 512: tb * 512 + N],
                                                                      start=True, stop=True), reads=[R_affT, R_oh], writes=[rp])
                k.op("act", lambda e, pt=pt, tb=tb, N=N: e.activation(out=bcs[:, tb * 512: tb * 512 + N], in_=pt[:, 0:N], func=AF.Copy),
                     reads=[rp], writes=[R_bcs])
            for tt in range(nt):
                gt = gt0 + tt
                tsl = slice(tt * 128, (tt + 1) * 128)
                k.op("dve", lambda e, tsl=tsl: e.scalar_tensor_tensor(out=dj, in0=bcs[:, tsl], scalar=1.0, in1=ident_f[:], op0=ALU.mult,
                                                                     op1=ALU.mult, accum_out=sm[:, 4:5]),
                     reads=[R_bcs, R_ident], writes=[R_cmpj, R_sm])
                k.op("dve", lambda e: e.tensor_scalar(out=cmpj[:, 0:T], in0=bcs[:, 0:T], scalar1=sm[:, 4:5], scalar2=None, op0=ALU.is_gt,
                                                      op1=ALU.add, accum_out=sm[:, 5:6]), reads=[R_bcs, R_sm], writes=[R_cmpj, R_sm])
                k.op("dve", lambda e, gt=gt, ex=ex: e.tensor_scalar(out=sel_all[:, gt, ex:ex + 1], in0=sm[:, 5:6], scalar1=float(cap) - 0.5,
                                                                    scalar2=None, op0=ALU.is_lt), reads=[R_sm], writes=[R_sel])
        for tt in range(nt):
            gt = gt0 + tt
            pt, rp = next_ps()
            for t2 in range(tt + 1):
                k.op("dve", lambda e, t2=t2: e.tensor_copy(out=selt[:], in_=sel_all[:, gt0 + t2, :]), reads=[R_sel], writes=[R_selt])
                k.op("pe", lambda e, pt=pt, t2=t2, tt=tt: e.matmul(pt[:, 0:NE], lhsT=onesUb[:, 0 if t2 < tt else 1, :], rhs=selt[:],
                                                                  start=(t2 == 0), stop=(t2 == tt)), reads=[R_selt, R_oUb], writes=[rp])
            pos1 = carve(20480, [128, NE], F32)
            k.op("dve", lambda e, pt=pt, pos1=pos1: e.tensor_scalar(out=pos1, in0=pt[:, 0:NE], scalar1=1.0, scalar2=None, op0=ALU.add),
                 reads=[rp], writes=[R_cmpj])
            k.op("dve", lambda e, gt=gt, pos1=pos1: e.tensor_tensor(out=pos1, in0=pos1, in1=sel_all[:, gt, :], op=ALU.mult),
                 reads=[R_sel], writes=[R_cmpj])
            k.op("dve", lambda e, gt=gt, pos1=pos1: e.tensor_scalar(out=posm_all[:, gt, :], in0=pos1, scalar1=-1.0, scalar2=None, op0=ALU.add),
                 reads=[R_cmpj], writes=[R_posm])
        k.barrier()

    wg_d = din("w_exp_gate", [NEXP, D, FF])
    wu_d = din("w_exp_up", [NEXP, D, FF])
    wd_d = din("w_exp_down", [NEXP, FF, D])
    caps = [2 * r_[0] // NE for r_ in reqs]
    sos = [sum(caps[:i_]) for i_ in range(len(reqs))]
    NSLOT = sum(caps)
    eo_scr = nc.dram_tensor("eo_scr", [NE, NSLOT, D], BF16).ap()
    R_eo = Reg("eo_scr")
    mtiles = []
    cur = [0, 0]
    for r_i in range(len(reqs)):
        if cur[1] + caps[r_i] > 128:
            mtiles.append(tuple(cur))
            cur = [sos[r_i], 0]
        cur[1] += caps[r_i]
    mtiles.append(tuple(cur))

    def moe():
        k.barrier()
        h2tm = carve(0, [128, NGT, D], BF16)
        R_h2tm = Reg("h2tm")
        k.dma("sp", h2tm, h2_scr.rearrange("(t p) f -> p t f", p=128), reads=[R_h2s], writes=[R_h2tm])
        xsT = carve(49152, [128, 16, NSLOT], BF16)
        hidT = carve(55296, [128, 32, NSLOT], BF16)
        wdb = [carve(67584 + i_ * 8192, [128, 32, 128], BF16) for i_ in range(2)]
        Sx = carve(83968, [128, NGT, 128], BF16)
        gsl = carve(87040, [128, 4], F32)
        sgt = carve(87104, [128, NSLOT], F32)
        eot = carve(88000, [128, 128], F32)
        R_xsT, R_hidT, R_Sx, R_gsl, R_sgt, R_eot = (Reg(n) for n in ("xsT", "hidT", "Sx", "gsl", "sgt", "eot"))
        R_wdb = [Reg("wdb0"), Reg("wdb1")]
        eoh = hT[:].rearrange("p a b -> p (a b)")[:, 0:len(mtiles) * D].rearrange("p (a b) -> p a b", b=D)
        R_eoh = R_hT
        wdi = 0
        for ex in range(NEXP):
            for r_i, (T, latent, cv, off) in enumerate(reqs):
                cap = caps[r_i]
                for tt in range(T // 128):
                    gt = off // 128 + tt
                    k.op("dve", lambda e, gt=gt, cap=cap, ex=ex: e.tensor_scalar(out=Sx[:, gt, 0:cap], in0=iota_row[:, 0:cap],
                                                                               scalar1=posm_all[:, gt, ex:ex + 1], scalar2=None,
                                                                               op0=ALU.is_equal), reads=[R_posm, R_moec], writes=[R_Sx])
            for r_i, (T, latent, cv, off) in enumerate(reqs):
                cap = caps[r_i]
                nt = T // 128
                gt0 = off // 128
                for k4 in range(4):
                    pt, rp = next_ps()

                    def mg(e, pt=pt, k4=k4, cap=cap, nt=nt, gt0=gt0):
                        ins = None
                        for j in range(4):
                            kc = k4 * 4 + j
                            for tt in range(nt):
                                ins = e.matmul(pt[:, j * cap:(j + 1) * cap], lhsT=h2tm[:, gt0 + tt, kc * 128:(kc + 1) * 128],
                                               rhs=Sx[:, gt0 + tt, 0:cap], start=(tt == 0), stop=(tt == nt - 1))
                        return ins
                    k.op("pe", mg, reads=[R_h2tm, R_Sx], writes=[rp])
                    k.op("act", lambda e, pt=pt, k4=k4, cap=cap, r_i=r_i: e.activation(
                        out=xsT[:, k4 * 4:(k4 + 1) * 4, sos[r_i]:sos[r_i] + cap], in_=pt[:, 0:4 * cap].rearrange("p (a b) -> p a b", b=cap),
                        func=AF.Copy), reads=[rp], writes=[R_xsT])
                mi = [i_ for i_, (s0, n_) in enumerate(mtiles) if s0 <= sos[r_i] < s0 + n_][0]
                po = sos[r_i] - mtiles[mi][0]
                pt, rp = next_ps()

                def mgs(e, pt=pt, cap=cap, nt=nt, gt0=gt0, po=po, ex=ex):
                    ins = None
                    for tt in range(nt):
                        ins = e.matmul(pt[po:po + cap, 0:2], lhsT=Sx[:, gt0 + tt, 0:cap], rhs=affhl[:, gt0 + tt, ex, :],
                                       start=(tt == 0), stop=(tt == nt - 1), tile_position=(0, po))
                    return ins
                k.op("pe", mgs, reads=[R_Sx, R_aff], writes=[rp])
                k.op("dve", lambda e, pt=pt, po=po, cap=cap, mi=mi: e.tensor_reduce(out=gsl[po:po + cap, mi:mi + 1], in_=pt[po:po + cap, 0:2],
                                                                                 op=ALU.add, axis=AX.X),
                     reads=[rp], writes=[R_gsl])
            for fc in range(FF // 128):
                wt, rw = next_w()
                wv = wt[:].rearrange("p (a kc f) -> p a kc f", a=2, f=128)
                k.dma("pool", wv[:, 0], wg_d[ex].rearrange("(kc p) f -> p kc f", p=128)[:, :, fc * 128:(fc + 1) * 128], writes=[rw])
                k.dma("pool", wv[:, 1], wu_d[ex].rearrange("(kc p) f -> p kc f", p=128)[:, :, fc * 128:(fc + 1) * 128], writes=[rw])
                pg, rpg = next_ps()
                pu, rpu = next_ps()

                def mgu(e, wv=wv, pg=pg, pu=pu):
                    ins = None
                    for a_, pp_ in ((0, pg), (1, pu)):
                        for kc in range(16):
                            ins = e.matmul(pp_[:, 0:NSLOT], lhsT=wv[:, a_, kc, :], rhs=xsT[:, kc, :], start=(kc == 0), stop=(kc == 15))
                    return ins
                k.op("pe", mgu, reads=[rw, R_xsT], writes=[rpg, rpu])
                k.op("act", lambda e, pg=pg: e.activation(out=sgt, in_=pg[:, 0:NSLOT], func=AF.Silu), reads=[rpg], writes=[R_sgt])
                k.op("dve", lambda e, pu=pu, fc=fc: e.tensor_tensor(out=hidT[:, fc, :], in0=sgt, in1=pu[:, 0:NSLOT], op=ALU.mult),
                     reads=[rpu, R_sgt], writes=[R_hidT])
            for m in range(D // 128):
                wd_, rwd = wdb[wdi], R_wdb[wdi]
                wdi = 1 - wdi
                k.dma("pool", wd_, wd_d[ex].rearrange("(fc p) d -> p fc d", p=128)[:, :, m * 128:(m + 1) * 128], writes=[rwd])
                for mi, (s0, n_) in enumerate(mtiles):
                    pt, rp = next_ps()

                    def mdn(e, pt=pt, wd_=wd_, s0=s0, n_=n_):
                        ins = None
                        for fc in range(FF // 128):
                            ins = e.matmul(pt[0:n_, 0:128], lhsT=hidT[:, fc, s0:s0 + n_], rhs=wd_[:, fc, :], start=(fc == 0),
                                           stop=(fc == FF // 128 - 1))
                        return ins
                    k.op("pe", mdn, reads=[rwd, R_hidT], writes=[rp])
                    k.op("dve", lambda e, pt=pt, n_=n_, mi=mi, m=m: e.tensor_scalar(out=eoh[0:n_, mi, m * 128:(m + 1) * 128], in0=pt[0:n_, 0:128],
                                                                                 scalar1=gsl[0:n_, mi:mi + 1], scalar2=None, op0=ALU.mult),
                         reads=[rp, R_gsl], writes=[R_eoh])
            for mi, (s0, n_) in enumerate(mtiles):
                k.dma("sp", eo_scr[ex, s0:s0 + n_, :], eoh[0:n_, mi, :], reads=[R_eoh], writes=[R_eo])
        k.barrier()

    def combine(r_i):
        T, latent, cv, off = reqs[r_i]
        cap = caps[r_i]
        nt = T // 128
        gt0 = off // 128
        k.barrier()
        Eo = carve(0, [128, NE, D], BF16)
        STe = carve(65536, [128, NE, 128], BF16)
        Sx1 = carve(69632, [128, 128], BF16)
        yt = carve(69888, [128, D], F32)
        g2row = carve(78080, [128, D], F32)
        x2t = carve(86272, [128, D], F32)
        gbm = carve(94464, [128, 128], F32)
        R_Eo, R_STe, R_Sx1, R_yt, R_g2, R_x2t, R_gbm = (Reg(n) for n in ("Eo", "STe", "Sx1", "yt", "g2row", "x2t", "gbm2"))
        with nc.allow_non_contiguous_dma(reason="per-expert row blocks"):
            k.dma("sp", Eo[0:cap, 0:NEXP, :], eo_scr[0:NEXP, sos[r_i]:sos[r_i] + cap, :].rearrange("e s d -> s e d"), reads=[R_eo], writes=[R_Eo])
        bcast_row(80, cv, g2row, R_g2, gbm, R_gbm)
        for tt in range(nt):
            gt = gt0 + tt
            rows = slice(off + tt * 128, off + (tt + 1) * 128)
            k.dma("sp", x2t, x2_scr[rows, :], reads=[R_x2], writes=[R_x2t])
            for ex in range(NEXP):
                k.op("dve", lambda e, gt=gt, ex=ex: e.tensor_scalar(out=Sx1[:, 0:cap], in0=iota_row[:, 0:cap], scalar1=posm_all[:, gt, ex:ex + 1],
                                                                    scalar2=None, op0=ALU.is_equal), reads=[R_posm, R_moec], writes=[R_Sx1])
                pt, rp = next_ps()
                k.op("pe", lambda e, pt=pt: e.matmul(pt[0:cap, 0:128], lhsT=Sx1[:, 0:cap], rhs=ident_b[:], start=True, stop=True),
                     reads=[R_Sx1, R_identb], writes=[rp])
                k.op("act", lambda e, pt=pt, ex=ex: e.activation(out=STe[0:cap, ex, :], in_=pt[0:cap, 0:128], func=AF.Copy),
                     reads=[rp], writes=[R_STe])
            for cbk in range(4):
                cols = slice(cbk * 512, (cbk + 1) * 512)
                pt, rp = next_ps()

                def msc(e, pt=pt, cols=cols):
                    ins = None
                    for ex in range(NEXP):
                        ins = e.matmul(pt[:, :], lhsT=STe[0:cap, ex, :], rhs=Eo[0:cap, ex, cols], start=(ex == 0), stop=(ex == NEXP - 1))
                    return ins
                k.op("pe", msc, reads=[R_STe, R_Eo], writes=[rp])
                k.op("dve", lambda e, pt=pt, cols=cols: e.tensor_tensor(out=yt[:, cols], in0=pt[:, :], in1=g2row[:, cols], op=ALU.mult),
                     reads=[rp, R_g2], writes=[R_yt])
            k.op("dve", lambda e: e.tensor_tensor(out=yt, in0=yt, in1=x2t, op=ALU.add), reads=[R_x2t], writes=[R_yt])
            k.op("act", lambda e: e.activation(out=x2t, in_=yt, func=AF.Square, accum_out=stat[:, 0:1]), reads=[R_yt], writes=[R_x2t, R_stat])
            k.op("dve", lambda e: e.tensor_scalar(out=stat[:, 1:2], in0=stat[:, 0:1], scalar1=1.0 / D, scalar2=EPS, op0=ALU.mult, op1=ALU.add),
                 reads=[R_stat], writes=[R_stat])
            k.op("act", lambda e: e.activation(out=stat[:, 2:3], in_=stat[:, 1:2], func=AF.Sqrt), reads=[R_stat], writes=[R_stat])
            k.op("dve", lambda e: e.reciprocal(out=stat[:, 3:4], in_=stat[:, 2:3]), reads=[R_stat], writes=[R_stat])
            k.op("dve", lambda e: e.scalar_tensor_tensor(out=yt, in0=yt, scalar=stat[:, 3:4], in1=gnw[:], op0=ALU.mult, op1=ALU.mult),
                 reads=[R_stat, R_gnw], writes=[R_yt])
            k.dma("sp", yout[rows, :], yt, reads=[R_yt], writes=[R_yout])

    R_yout = Reg("yout")
    dbg_cur = [False]
    if not cfg.get('skip_s5'):
        s5_precompute()
    pidx = 0
    for ri, (T, latent, cv, off) in enumerate(reqs):
        nt = T // 128
        k.barrier()
        norm_to_T(lambda tt, off=off: xin[off + tt * 128: off + (tt + 1) * 128, :], nt, 0, 0, cv, hT, R_hT)
        k.barrier()
        dbg_cur[0] = (ri == dbg_req)
        if not cfg.get("skip_s5") and cfg.get("s5_mode") != "pre":
            s5_branch(T, latent, pidx)
        if cfg.get("skip_ret"):
            continue
        retention(T, latent, off, pidx)
        if not latent:
            pidx += 1
        if cfg.get("stop") == "ret":
            continue
        merge_out(T, cv, off)
        norm2_router(T, cv, off, 2 * T // NE)
        if "o_tm" in dbg and ri == dbg_req:
            R_d = Reg("dbgo")
            k.dma("sp", dbg_outs["o_tm"].rearrange("(t p) f -> p t f", p=128), o_tm[:, 0:nt, :], reads=[R_o], writes=[R_d])
            out_regs.append(R_d)
    out_regs.append(out_regs_ret)
    if not cfg.get("skip_s5"):
        out_regs.append(R_news5)
    if cfg.get("stop") is None or cfg.get("stop") == "all":
        moe()
        with nc.allow_non_contiguous_dma(reason="partition-broadcast param loads"):
            k.dma("sp", gnw[:], fnorm_d.partition_broadcast(128), reads=[R_gnw], writes=[R_gnw])
        for r_i in range(len(reqs)):
            combine(r_i)
        out_regs.append(R_yout)

    k.finish(out_regs)
    return nc


def _ret_consts():
    j = np.arange(128)[:, None].astype(np.float32)
    i = np.arange(128)[None, :].astype(np.float32)
    p = np.arange(128, dtype=np.float32)[:, None]
    return np.concatenate([np.maximum(i - j, 0), np.maximum(j - i, 0), (i >= j).astype(np.float32),
                           (j > i).astype(np.float32), p + 1, 128 - p, 127 - p, p], 1).astype(np.float32)


def _rope_tabs():
    t = np.arange(1024)
    row, col = t // 64, t % 64
    d = np.arange(128)
    freq = (10000.0 ** (-(np.arange(32, dtype=np.float32)) / 32)).astype(np.float32)
    pos = np.where((d < 64)[:, None], row[None, :], col[None, :]).astype(np.float32)
    ang = (pos * freq[d % 32][:, None]).astype(np.float32)
    sgn = np.where((d % 64) < 32, -1.0, 1.0)[:, None]
    return np.concatenate([np.cos(ang), np.sin(ang) * sgn], 1).astype(np.float32)


def _rope_psw():
    P = np.zeros((128, 128), np.float32)
    for d in range(128):
        P[d + 32 if (d % 64) < 32 else d - 32, d] = 1.0
    return P


def _s5_rowmask():
    par = (np.arange(128) // 16) % 2
    m = np.zeros((128, 4), np.float32)
    m[:, 0] = (par == 0)
    m[:, 1] = (par == 1)
    m[:, 2] = -m[:, 0]
    m[:, 3] = -m[:, 1]
    return m


def _moe_consts():
    m = np.zeros((128, 384), np.float32)
    m[:, 0:128] = np.arange(128, dtype=np.float32)[None, :]
    m[:, 128:256] = 1.0
    m[:, 256:384] = (np.arange(128)[:, None] < np.arange(128)[None, :]).astype(np.float32)
    return m


def kernel(**inputs):
    f = {k_: np.ascontiguousarray(np.asarray(v)) for k_, v in inputs.items()}
    B = f["x_prompt"].shape[0]
    BS = f["x_sample"].shape[0]
    npr = B // NCORES
    assert BS == NCORES and npr == 2
    TP, TS = f["x_prompt"].shape[1], f["x_sample"].shape[1]
    reqs = [(TP, False, 0, TP * i) for i in range(npr)] + [(TS, True, 1, TP * npr)]
    cfg = dict(reqs=reqs, ncv=2, n_prompt=npr)
    nc = build(cfg)
    common = dict(ident=np.eye(128, dtype=np.float32), w_ada=f["w_ada"][0], b_ada=f["b_ada"][0],
                  norm1=f["norm1"][0], norm2=f["norm2"][0], final_norm=f["final_norm"], w_in=f["w_in"][0],
                  ret_decay_logit=f["ret_decay_logit"][0].reshape(16), ret_gn_w=f["ret_gn_w"][0],
                  ret_consts=_ret_consts(), rope_tabs=_rope_tabs(), rope_psw=_rope_psw(),
                  s5_a_re=f["s5_a_re"][0], s5_a_im=f["s5_a_im"][0], s5_log_dt=f["s5_log_dt"][0], s5_b_re=f["s5_b_re"][0],
                  s5_b_im=f["s5_b_im"][0], s5_c_re=f["s5_c_re"][0], s5_c_im=f["s5_c_im"][0], s5_d=f["s5_d"][0],
                  s5_rowmask=_s5_rowmask(), w_s5_glu=f["w_s5_glu"][0], w_ret_out=f["w_ret_out"][0], w_out=f["w_out"][0],
                  w_router=f["w_router"][0], moe_consts=_moe_consts(),
                  w_exp_gate=f["w_exp_gate"][0], w_exp_up=f["w_exp_up"][0], w_exp_down=f["w_exp_down"][0])
    in_maps = []
    for c in range(NCORES):
        m = dict(common)
        m["xin"] = np.concatenate([f["x_prompt"][c * npr:(c + 1) * npr].reshape(npr * TP, D), f["x_sample"][c]], 0)
        cv = np.stack([f["c_ctx"], f["c"][c]], 0)
        m["cT"] = np.ascontiguousarray(cv.T.reshape(16, 128, 2).transpose(1, 0, 2).reshape(128, 32))
        m["state_ret"] = f["state_ret"][c, 0]
        m["state_s5_re"] = f["state_s5_re"][c, 0]
        m["state_s5_im"] = f["state_s5_im"][c, 0]
        in_maps.append(m)
    res = run_bass_kernel_spmd(nc, in_maps, core_ids=list(range(NCORES)))
    ys = [r["y"] for r in res.results]
    y_prompt = np.concatenate([y[:npr * TP].reshape(npr, TP, D) for y in ys], 0)
    y_sample = np.stack([y[npr * TP:] for y in ys], 0)
    new_re = np.concatenate([r["new_s5_re"] for r in res.results], 0)[:, None]
    new_im = np.concatenate([r["new_s5_im"] for r in res.results], 0)[:, None]
    new_ret = np.concatenate([r["new_ret"] for r in res.results], 0)[:, None]
    return (y_prompt.astype(np.float32), y_sample.astype(np.float32), new_re.astype(np.float32),
            new_im.astype(np.float32), new_ret.astype(np.float32))
```

```python
import numpy as np
import concourse.bass as bass
import concourse.mybir as mybir
from concourse.bass_utils import run_bass_kernel_spmd

F32 = mybir.dt.float32
BF16 = mybir.dt.bfloat16
ALU = mybir.AluOpType
AF = mybir.ActivationFunctionType
AX = mybir.AxisListType

D = 2048
NCORES = 8
EPS = 1e-6
S5W = 1024
NG = 64
NP_ = 64
NH = 8
DK = 128
DV = 256
NE = 16
FF = 4096
IN_COLS = 11264
C_U, C_Q, C_K, C_V, C_G, C_GA, C_GB = 0, 1024, 2048, 3072, 5120, 7168, 9216


SAME_ENGINE_INORDER = False


class Reg:
    __slots__ = ("name", "w", "r", "sem", "cnt")

    def __init__(self, name):
        self.name = name
        self.w = None
        self.r = {}
        self.sem = None
        self.cnt = 0


class K:
    def __init__(self, nc):
        self.nc = nc
        self.eng = {"pe": nc.tensor, "dve": nc.vector, "act": nc.scalar, "pool": nc.gpsimd, "sp": nc.sync}
        self.sem = {}
        self.cnt = {}
        self.seen = {e: {} for e in self.eng}
        self.nsem = 0
        self.dma_toks = {}
        for e in self.eng:
            self._newsem(e)

    def _mk(self, name):
        self.nsem += 1
        return self.nc.semaphore(f"{name}_{self.nsem}").__enter__()

    def _newsem(self, e):
        self.sem[e] = self._mk(e)
        self.cnt[e] = 0

    def _wait(self, e, tok):
        if tok is None:
            return
        sem, val, src = tok
        if src == e and (e == "pe" or SAME_ENGINE_INORDER):
            return
        d = self.seen[e]
        key = id(sem)
        if d.get(key, (None, 0))[1] >= val:
            return
        self.eng[e].wait_ge(sem, val)
        d[key] = (sem, val)

    def _deps(self, e, reads, writes):
        for r in reads:
            self._wait(e, r.w)
        for r in writes:
            self._wait(e, r.w)
            for t in r.r.values():
                self._wait(e, t)

    def _commit(self, tok, reads, writes):
        for r in writes:
            r.w = tok
            r.r = {}
        for r in reads:
            r.r[tok[2]] = tok

    def op(self, e, fn, reads=(), writes=()):
        self._deps(e, reads, writes)
        ins = fn(self.eng[e])
        if self.cnt[e] > 30000:
            self._newsem(e)
        self.cnt[e] += 1
        ins.then_inc(self.sem[e], 1)
        tok = (self.sem[e], self.cnt[e], e)
        self._commit(tok, reads, writes)
        return tok

    def dma(self, e, out, in_, reads=(), writes=()):
        self._deps(e, reads, writes)
        r0 = writes[0]
        if r0.sem is None or r0.cnt > 1800:
            r0.sem = self._mk("d")
            r0.cnt = 0
        r0.cnt += 1
        self.eng[e].dma_start(out=out, in_=in_).then_inc(r0.sem, 16)
        tok = (r0.sem, 16 * r0.cnt, "dma_" + e + r0.name)
        self.dma_toks[id(r0.sem)] = tok
        self._commit(tok, reads, writes)
        return tok

    def barrier(self):
        toks = [(self.sem[e], self.cnt[e], e) for e in self.eng if self.cnt[e] > 0]
        toks += list(self.dma_toks.values())
        for e in self.eng:
            for t in toks:
                if t[2] == e:
                    continue
                sem, val, src = t
                d = self.seen[e]
                if d.get(id(sem), (None, 0))[1] >= val:
                    continue
                self.eng[e].wait_ge(sem, val)
                d[id(sem)] = (sem, val)

    def finish(self, regs):
        for r in regs:
            self._wait("sp", r.w)


def build(cfg):
    nc = bass.Bass("TRN2", target_bir_lowering=False)
    k = K(nc)
    reqs = cfg["reqs"]
    NTOK = sum(r[0] for r in reqs)
    NCV = cfg["ncv"]
    NEXP = cfg.get("nexp", NE)
    dbg = cfg.get("dbg", {})
    stop = cfg.get("stop")
    dbg_req = cfg.get("dbg_req", 0)

    def din(name, shape, dt=F32):
        return nc.dram_tensor(name, list(shape), dt, kind="ExternalInput").ap()

    def dout(name, shape, dt=F32):
        return nc.dram_tensor(name, list(shape), dt, kind="ExternalOutput").ap()

    def sb(name, shape, dt=F32):
        return nc.sbuf_tensor(name, list(shape), dt).__enter__()

    def ps(name, shape, dt=F32):
        return nc.psum_tensor(name, list(shape), dt).__enter__()

    xin = din("xin", [NTOK, D])
    cT_d = din("cT", [128, 16 * NCV])
    ident_d = din("ident", [128, 128])
    w_ada = din("w_ada", [D, 6 * D])
    b_ada = din("b_ada", [6 * D])
    norm1_d = din("norm1", [D])
    norm2_d = din("norm2", [D])
    fnorm_d = din("final_norm", [D])
    w_in = din("w_in", [D, IN_COLS])
    yout = dout("y", [NTOK, D])
    dbg_outs = {}
    for name, shape in dbg.items():
        if name.startswith("_"):
            continue
        dbg_outs[name] = dout("dbg_" + name, shape, BF16 if name == "o_tm" else F32)
    out_regs = []

    ident_f = sb("ident_f", [128, 128])
    ident_b = sb("ident_b", [128, 128], BF16)
    R_ident = Reg("ident")
    k.dma("sp", ident_f[:], ident_d[:], writes=[R_ident])
    R_identb = Reg("identb")
    k.op("dve", lambda e: e.tensor_copy(out=ident_b[:], in_=ident_f[:]), reads=[R_ident], writes=[R_identb])

    PS = [ps(f"psb{i}", [128, 512]) for i in range(8)]
    R_PS = [Reg(f"ps{i}") for i in range(8)]
    ps_rr = [0]

    def next_ps():
        i = ps_rr[0]
        ps_rr[0] = (i + 1) % 8
        return PS[i], R_PS[i]

    mod = sb("mod", [128, 96, NCV])
    R_mod = Reg("mod")
    cT = sb("cT_sb", [128, 16 * NCV])
    sT = sb("sT", [128, 16, NCV], BF16)
    R_cT = Reg("cT")
    R_sT = Reg("sT")
    k.dma("sp", cT[:], cT_d[:], writes=[R_cT])
    k.op("act", lambda e: e.activation(out=sT[:].rearrange("p a b -> p (a b)"), in_=cT[:], func=AF.Silu),
         reads=[R_cT], writes=[R_sT])
    bada = sb("bada", [128, 96])
    R_bada = Reg("bada")
    with nc.allow_non_contiguous_dma(reason="small one-time param loads"):
        k.dma("sp", bada[:], b_ada.rearrange("(c p) -> p c", p=128), writes=[R_bada])
        n1 = sb("n1", [128, 16])
        n2 = sb("n2", [128, 16])
        R_n = Reg("n12")
        k.dma("sp", n1[:], norm1_d.rearrange("(c p) -> p c", p=128), writes=[R_n])
        k.dma("sp", n2[:], norm2_d.rearrange("(c p) -> p c", p=128), writes=[R_n])

    WSLOT_BYTES = 16 * 512 * 2
    WB = 256
    wslots = [sb(f"wslot{i}", [128, 16 * WB], BF16) for i in range(2)]
    R_w = [Reg(f"w{i}") for i in range(2)]
    w_rr = [0]

    def next_w():
        i = w_rr[0]
        w_rr[0] = 1 - i
        return wslots[i], R_w[i]

    w_ada_v = w_ada.rearrange("(kc p) f -> p kc f", p=128)
    for fb in range(6 * D // WB):
        wt, rw = next_w()
        wv = wt[:].rearrange("p (kc f) -> p kc f", f=WB)
        k.dma("pool", wv, w_ada_v[:, :, fb * WB:(fb + 1) * WB], writes=[rw])
        pt, rp = next_ps()

        def mm(e, wv=wv, pt=pt):
            ins = None
            for fc in range(WB // 128):
                for kc in range(16):
                    ins = e.matmul(pt[:, fc * NCV:(fc + 1) * NCV], lhsT=wv[:, kc, fc * 128:(fc + 1) * 128],
                                   rhs=sT[:, kc, :], start=(kc == 0), stop=(kc == 15))
            return ins
        k.op("pe", mm, reads=[rw, R_sT], writes=[rp])
        for fc in range(WB // 128):
            idx = fb * (WB // 128) + fc
            k.op("dve", lambda e, idx=idx, fc=fc, pt=pt: e.tensor_scalar(
                out=mod[:, idx, :], in0=pt[:, fc * NCV:(fc + 1) * NCV], scalar1=bada[:, idx:idx + 1], scalar2=None,
                op0=ALU.add), reads=[rp, R_bada], writes=[R_mod])
    g12 = sb("g12", [128, 2, 16, NCV])
    R_g = Reg("g12")
    for j, (nn, s) in enumerate(((n1, 1), (n2, 4))):
        for m in range(16):
            k.op("dve", lambda e, j=j, m=m, nn=nn, s=s: e.tensor_scalar(
                out=g12[:, j, m, :], in0=mod[:, 16 * s + m, :], scalar1=1.0, scalar2=nn[:, m:m + 1],
                op0=ALU.add, op1=ALU.mult), reads=[R_mod, R_n], writes=[R_g])

    if "mod" in dbg:
        R_d = Reg("dbgmod")
        k.dma("sp", dbg_outs["mod"].rearrange("p (a b) -> p a b", b=NCV), mod[:], reads=[R_mod], writes=[R_d])
        out_regs.append(R_d)

    TMAX = max(r[0] for r in reqs)
    hT = sb("hT", [128, 16, TMAX], BF16)
    R_hT = Reg("hT")
    xts = [sb("xt0", [128, D])] * 2
    R_xt = [Reg("xt0")] * 2
    R_junk = Reg("junk")
    stat = sb("stat", [128, 8])
    R_stat = Reg("stat")
    xt_rr = [0]

    def norm_to_T(src_ap_fn, ntiles, gsel, shift_s, cv, dstT, R_dst, R_src_fn=None):
        for tt in range(ntiles):
            i = xt_rr[0]
            xt_rr[0] = 1 - i
            xt, rx = xts[i], R_xt[i]
            k.dma("sp", xt[:], src_ap_fn(tt), reads=([R_src_fn] if R_src_fn else []), writes=[rx])
            k.op("act", lambda e, xt=xt: e.activation(out=junk, in_=xt[:], func=AF.Square, accum_out=stat[:, 0:1]),
                 reads=[rx], writes=[R_junk, R_stat])
            k.op("dve", lambda e: e.tensor_scalar(out=stat[:, 1:2], in0=stat[:, 0:1], scalar1=1.0 / D, scalar2=EPS,
                                                  op0=ALU.mult, op1=ALU.add), reads=[R_stat], writes=[R_stat])
            k.op("act", lambda e: e.activation(out=stat[:, 2:3], in_=stat[:, 1:2], func=AF.Sqrt),
                 reads=[R_stat], writes=[R_stat])
            k.op("dve", lambda e: e.reciprocal(out=stat[:, 3:4], in_=stat[:, 2:3]), reads=[R_stat], writes=[R_stat])
            k.op("dve", lambda e, xt=xt: e.tensor_scalar(out=xt[:], in0=xt[:], scalar1=stat[:, 3:4], scalar2=None,
                                                         op0=ALU.mult), reads=[rx, R_stat], writes=[rx])
            for q4 in range(4):
                pt, rp = next_ps()

                def tr(e, q4=q4, pt=pt, xt=xt):
                    ins = None
                    for j in range(4):
                        m = q4 * 4 + j
                        ins = e.transpose(pt[:, j * 128:(j + 1) * 128], xt[:, m * 128:(m + 1) * 128], ident_f[:])
                    return ins
                k.op("pe", tr, reads=[rx, R_ident], writes=[rp])
                for j in range(4):
                    m = q4 * 4 + j
                    k.op("act", lambda e, m=m, j=j, pt=pt, tt=tt: e.activation(
                        out=dstT[:, m, tt * 128:(tt + 1) * 128], in_=pt[:, j * 128:(j + 1) * 128], func=AF.Identity,
                        scale=g12[:, gsel, m, cv:cv + 1], bias=mod[:, 16 * shift_s + m, cv:cv + 1]),
                        reads=[rp, R_g, R_mod], writes=[R_dst])


    NPR = cfg.get("n_prompt", 2)
    w_in_v = w_in.rearrange("(kc p) f -> p kc f", p=128)
    dlog_d = din("ret_decay_logit", [16])
    gnw_d = din("ret_gn_w", [D])
    cst_d = din("ret_consts", [128, 4 * 128 + 4])
    rope_d = din("rope_tabs", [128, 2 * 1024])
    psw_d = din("rope_psw", [128, 128])
    sret_d = din("state_ret", [2, NH, DK, DV])
    newret = dout("new_ret", [NPR, 2, NH, DK, DV])
    out_regs_ret = Reg("newret")

    cst = sb("cst", [128, 4 * 128 + 4])
    R_cst = Reg("cst")
    k.dma("sp", cst[:], cst_d[:], writes=[R_cst])
    psw = sb("psw", [128, 128])
    R_psw = Reg("psw")
    k.dma("sp", psw[:], psw_d[:], writes=[R_psw])
    gnw = sb("gnw", [128, D])
    R_gnw = Reg("gnw")
    lg = sb("lg", [128, 16])
    R_lg = Reg("lg")
    with nc.allow_non_contiguous_dma(reason="partition-broadcast param loads"):
        k.dma("sp", gnw[:], gnw_d.partition_broadcast(128), writes=[R_gnw])
        k.dma("sp", lg[:], dlog_d.partition_broadcast(128), writes=[R_lg])
    k.op("act", lambda e: e.activation(out=lg[:], in_=lg[:], func=AF.Exp, scale=-1.0), reads=[R_lg], writes=[R_lg])
    k.op("dve", lambda e: e.tensor_scalar(out=lg[:], in0=lg[:], scalar1=1.0, scalar2=None, op0=ALU.add),
         reads=[R_lg], writes=[R_lg])
    k.op("act", lambda e: e.activation(out=lg[:], in_=lg[:], func=AF.Ln), reads=[R_lg], writes=[R_lg])
    k.op("dve", lambda e: e.tensor_scalar(out=lg[:], in0=lg[:], scalar1=-1.0, scalar2=None, op0=ALU.mult),
         reads=[R_lg], writes=[R_lg])
    dec = sb("dec", [128, 3, 16])
    R_dec = Reg("dec")
    for (row, col, sl) in ((0, 512, slice(0, 8)), (0, 513, slice(8, 16)), (1, 514, slice(0, 8)), (1, 515, slice(8, 16))):
        k.op("act", lambda e, row=row, col=col, sl=sl: e.activation(out=dec[:, row, sl], in_=lg[:, sl], func=AF.Exp,
                                                                    scale=cst[:, col:col + 1]),
             reads=[R_lg, R_cst], writes=[R_dec])
    k.op("act", lambda e: e.activation(out=dec[:, 2, :], in_=lg[:], func=AF.Exp, scale=128.0), reads=[R_lg], writes=[R_dec])
    MT = sb("MT", [128, NH, 128])
    mtmp = sb("mtmp", [128, 128])
    R_MT = Reg("MT")
    R_mtmp = Reg("mtmp")
    for h in range(NH):
        k.op("act", lambda e, h=h: e.activation(out=MT[:, h, :], in_=cst[:, 0:128], func=AF.Exp, scale=lg[:, h:h + 1]),
             reads=[R_lg, R_cst], writes=[R_MT])
        k.op("dve", lambda e, h=h: e.tensor_tensor(out=MT[:, h, :], in0=MT[:, h, :], in1=cst[:, 256:384], op=ALU.mult),
             reads=[R_cst], writes=[R_MT])
        k.op("act", lambda e, h=h: e.activation(out=mtmp[:], in_=cst[:, 128:256], func=AF.Exp, scale=lg[:, 8 + h:9 + h]),
             reads=[R_lg, R_cst], writes=[R_mtmp])
        k.op("dve", lambda e, h=h: e.tensor_tensor(out=mtmp[:], in0=mtmp[:], in1=cst[:, 384:512], op=ALU.mult),
             reads=[R_cst], writes=[R_mtmp])
        k.op("dve", lambda e, h=h: e.tensor_tensor(out=MT[:, h, :], in0=MT[:, h, :], in1=mtmp[:], op=ALU.add),
             reads=[R_mtmp], writes=[R_MT])

    def linear_fm(wview, KC, col0, ncols, rhsT, R_rhs, T, epi):
        for cb in range(ncols // WB):
            wt, rw = next_w()
            wv = wt[:, 0:KC * WB].rearrange("p (kc f) -> p kc f", f=WB)
            k.dma("pool", wv, wview[:, :, col0 + cb * WB: col0 + (cb + 1) * WB], writes=[rw])
            for c4 in range(WB // 128):
                for tb in range((T + 511) // 512):
                    N = min(512, T - tb * 512)
                    pt, rp = next_ps()

                    def mm(e, wv=wv, pt=pt, c4=c4, tb=tb, N=N):
                        ins = None
                        for kc in range(KC):
                            ins = e.matmul(pt[:, 0:N], lhsT=wv[:, kc, c4 * 128:(c4 + 1) * 128],
                                           rhs=rhsT[:, kc, tb * 512: tb * 512 + N], start=(kc == 0), stop=(kc == KC - 1))
                        return ins
                    k.op("pe", mm, reads=[rw, R_rhs], writes=[rp])
                    epi(pt, rp, cb * (WB // 128) + c4, tb, N)

    def linear_tm(wview, KC, col0, ncols, lhsT, R_lhs, T, epi):
        for cb in range(ncols // WB):
            wt, rw = next_w()
            wv = wt[:, 0:KC * WB].rearrange("p (kc f) -> p kc f", f=WB)
            k.dma("pool", wv, wview[:, :, col0 + cb * WB: col0 + (cb + 1) * WB], writes=[rw])
            for tt in range(T // 128):
                pt, rp = next_ps()

                def mm(e, wv=wv, pt=pt, tt=tt):
                    ins = None
                    for kc in range(KC):
                        ins = e.matmul(pt[:, 0:WB], lhsT=lhsT[:, kc, tt * 128:(tt + 1) * 128], rhs=wv[:, kc, :],
                                       start=(kc == 0), stop=(kc == KC - 1))
                    return ins
                k.op("pe", mm, reads=[rw, R_lhs], writes=[rp])
                epi(pt, rp, cb, tt)

    NTM = TMAX // 128
    arena = sb("arena", [128, 24576])

    def carve(off_bytes, shape, dt):
        n = 1
        for d_ in shape[1:]:
            n *= d_
        nbytes = n * (2 if dt == BF16 else 4)
        v = arena[:, off_bytes // 4:(off_bytes + nbytes) // 4]
        if dt != F32:
            v = v.bitcast(dt)
        if len(shape) == 3:
            v = v.rearrange("p (a b) -> p a b", b=shape[2])
        return v
    junk = carve(90112, [128, D], BF16)
    qT = carve(0, [128, NH, 1024], BF16)
    kT = carve(16384, [128, NH, 1024], BF16)
    R_qT = Reg("qT")
    R_kT = Reg("kT")
    v_tm = carve(32768, [128, 8, D], BF16)
    R_v = Reg("v_tm")
    o_tm = carve(65536, [128, 8, D], BF16)
    ropet = carve(65536, [128, 2, 1024], F32)
    R_o = Reg("o_tm")
    rtmp = xts[0][:, 0:512]
    R_rtmp = R_xt[0]
    rt1 = xts[0][:, 512:1024]
    R_rt1 = R_xt[0]
    Sst = sb("Sst", [128, DV])
    R_S = Reg("Sst")
    Sfb = sb("Sfb", [128, NTM, DV], BF16)
    Sbb = sb("Sbb", [128, NTM, DV], BF16)
    R_Sfb = Reg("Sfb")
    R_Sbb = Reg("Sbb")
    kd = sb("kd", [128, 128], BF16)
    R_kd = Reg("kd")
    Amat = sb("Amat", [128, 128], BF16)
    R_A = Reg("Amat")
    ot = sb("ot", [128, DV])
    ot2 = sb("ot2", [128, DV])
    R_ot = Reg("ot")
    R_ot2 = Reg("ot2")
    gst = sb("gst", [128, 8])
    R_gst = Reg("gst")
    sg = sb("sg", [128, 512], BF16)
    R_sg = Reg("sg")

    a_re_d = din("s5_a_re", [2, NG, NP_])
    a_im_d = din("s5_a_im", [2, NG, NP_])
    ldt_d = din("s5_log_dt", [2, NG])
    b_re_d = din("s5_b_re", [2, NG, NP_, 16])
    b_im_d = din("s5_b_im", [2, NG, NP_, 16])
    c_re_d = din("s5_c_re", [2, NG, 16, NP_])
    c_im_d = din("s5_c_im", [2, NG, 16, NP_])
    s5d_d = din("s5_d", [S5W])
    rowmask_d = din("s5_rowmask", [128, 4])
    h0re_d = din("state_s5_re", [2, NG, NP_])
    h0im_d = din("state_s5_im", [2, NG, NP_])
    news5 = [dout("new_s5_re", [NPR, 2, NG, NP_]), dout("new_s5_im", [NPR, 2, NG, NP_])]
    R_news5 = Reg("news5")
    btp_scr = nc.dram_tensor("btp_scr", [128, 32 * 128], BF16).ap()
    ctp_scr = nc.dram_tensor("ctp_scr", [128, 128 * 32], BF16).ap()
    R_scr = Reg("s5scr")
    TB = 32
    AAt = sb("AAt", [128, 2, 64])
    BBt = sb("BBt", [128, 2, 64])
    R_AB = Reg("AABB")
    dsk = sb("dsk", [128, 8])
    R_dsk = Reg("dsk")
    negpi = sb("negpi", [128, 1])
    R_negpi = Reg("negpi")
    rowmask = sb("rowmask", [128, 4])
    R_rowmask = Reg("rowmask")
    Xst = sb("Xst", [128, 2, 128])
    R_X = Reg("Xst")
    st1 = sb("st1", [128, 128])
    st2 = sb("st2", [128, 128])
    R_st1 = Reg("st1")
    R_st2 = Reg("st2")
    gyT = sb("gyT", [128, 8, TMAX], BF16)
    R_gyT = Reg("gyT")
    k.op("dve", lambda e: e.memset(negpi[:], -float(np.pi * (1 - 2e-6))), writes=[R_negpi])
    k.dma("sp", rowmask[:], rowmask_d[:], writes=[R_rowmask])
    with nc.allow_non_contiguous_dma(reason="small one-time param loads"):
        k.dma("sp", dsk[:], s5d_d.rearrange("(G p) -> p G", p=128), writes=[R_dsk])

    def s5_precompute():
        R_t = Reg("s5tmp")
        rows = {}
        names = ["ar", "ai", "lr", "li", "er", "ts", "tf", "fr", "sn", "cs", "Ar", "Ai", "nr", "den", "qr", "qi", "w1", "w2"]
        for n_i, nm in enumerate(names):
            rows[nm] = carve(n_i * 512, [64, 128], F32)[0:64, :]
        ti = carve(18 * 512, [64, 128], F32)[0:64, :].bitcast(mybir.dt.int32)
        ldt = carve(19 * 512, [64, 2], F32)[0:64, :]
        Pq = carve(10240, [128, 4, 64], F32)
        Bl = [carve(12288, [128, 64, 16], F32), carve(16384, [128, 64, 16], F32)]
        Bb = [carve(20480, [128, 64, 16], F32), carve(24576, [128, 64, 16], F32)]
        Bt = [carve(28672, [128, 64, 16], F32), carve(32768, [128, 64, 16], F32)]
        BQ = [carve(36864, [128, 16, 128], F32), carve(45056, [128, 16, 128], F32)]
        Cin = [carve(53248, [128, 16, 128], F32), carve(61440, [128, 16, 128], F32)]
        BTp_sb = carve(69632, [128, 32, 128], BF16)
        CTp_sb = carve(77824, [128, 128, 32], BF16)
        k.dma("sp", rows["ar"], a_re_d.rearrange("d (G r) p -> (d G) (r p)", r=2), writes=[R_t])
        k.dma("sp", rows["ai"], a_im_d.rearrange("d (G r) p -> (d G) (r p)", r=2), writes=[R_t])
        k.dma("sp", ldt, ldt_d.rearrange("d (G r) -> (d G) r", r=2), writes=[R_t])

        def T1(eng, fn):
            k.op(eng, fn, reads=[R_negpi], writes=[R_t])
        T1("act", lambda e: e.activation(out=ldt, in_=ldt, func=AF.Exp))
        for par in range(2):
            sl = slice(par * 64, (par + 1) * 64)
            T1("dve", lambda e, sl=sl, par=par: e.tensor_scalar(out=rows["lr"][:, sl], in0=rows["ar"][:, sl],
                                                              scalar1=ldt[:, par:par + 1], scalar2=None, op0=ALU.mult))
            T1("dve", lambda e, sl=sl, par=par: e.tensor_scalar(out=rows["li"][:, sl], in0=rows["ai"][:, sl],
                                                              scalar1=ldt[:, par:par + 1], scalar2=None, op0=ALU.mult))
        T1("act", lambda e: e.activation(out=rows["er"], in_=rows["lr"], func=AF.Exp))
        for (shift, dst) in ((0.5, "sn"), (0.75, "cs")):
            T1("dve", lambda e, shift=shift: e.tensor_scalar(out=rows["ts"], in0=rows["li"], scalar1=1.0 / (2 * np.pi),
                                                            scalar2=shift, op0=ALU.mult, op1=ALU.add))
            T1("dve", lambda e: e.tensor_copy(out=ti, in_=rows["ts"]))
            T1("dve", lambda e: e.tensor_copy(out=rows["tf"], in_=ti))
            T1("dve", lambda e: e.tensor_tensor(out=rows["w1"], in0=rows["tf"], in1=rows["ts"], op=ALU.is_gt))
            T1("dve", lambda e: e.tensor_tensor(out=rows["tf"], in0=rows["tf"], in1=rows["w1"], op=ALU.subtract))
            T1("dve", lambda e: e.tensor_tensor(out=rows["fr"], in0=rows["ts"], in1=rows["tf"], op=ALU.subtract))
            T1("act", lambda e, dst=dst: e.activation(out=rows[dst], in_=rows["fr"], func=AF.Sin, scale=float(2 * np.pi * (1 - 2e-6)),
                                                     bias=negpi[0:64, :]))
        TT = lambda o, a, b, op: T1("dve", lambda e: e.tensor_tensor(out=rows[o], in0=rows[a], in1=rows[b], op=op))
        TT("Ar", "er", "cs", ALU.mult)
        TT("Ai", "er", "sn", ALU.mult)
        T1("dve", lambda e: e.tensor_scalar(out=rows["nr"], in0=rows["Ar"], scalar1=-1.0, scalar2=None, op0=ALU.add))
        TT("w1", "ar", "ar", ALU.mult)
        TT("w2", "ai", "ai", ALU.mult)
        TT("den", "w1", "w2", ALU.add)
        T1("dve", lambda e: e.reciprocal(out=rows["den"], in_=rows["den"]))
        TT("w1", "nr", "ar", ALU.mult)
        TT("w2", "Ai", "ai", ALU.mult)
        TT("w1", "w1", "w2", ALU.add)
        TT("qr", "w1", "den", ALU.mult)
        TT("w1", "Ai", "ar", ALU.mult)
        TT("w2", "nr", "ai", ALU.mult)
        TT("w1", "w1", "w2", ALU.subtract)
        TT("qi", "w1", "den", ALU.mult)
        pt, rp = next_ps()

        def trq(e):
            ins = None
            for j, nm in enumerate(("Ar", "Ai", "qr", "qi")):
                ins = e.transpose(pt[:, j * 64:(j + 1) * 64], rows[nm], ident_f[0:64, 0:64])
            return ins
        k.op("pe", trq, reads=[R_t, R_ident], writes=[rp])
        k.op("dve", lambda e: e.tensor_copy(out=Pq.rearrange("p a b -> p (a b)"), in_=pt[:, 0:256]), reads=[rp], writes=[R_t])
        k.op("dve", lambda e: e.tensor_copy(out=AAt[:, 0, :], in_=Pq[:, 0, :]), reads=[R_t], writes=[R_AB])
        k.op("dve", lambda e: e.tensor_copy(out=AAt[:, 1, :], in_=Pq[:, 0, :]), reads=[R_t], writes=[R_AB])
        k.op("dve", lambda e: e.tensor_copy(out=BBt[:, 1, :], in_=Pq[:, 1, :]), reads=[R_t], writes=[R_AB])
        k.op("dve", lambda e: e.tensor_scalar(out=BBt[:, 0, :], in0=Pq[:, 1, :], scalar1=-1.0, scalar2=None, op0=ALU.mult),
             reads=[R_t], writes=[R_AB])
        with nc.allow_non_contiguous_dma(reason="64B-run param loads"):
            for ri_, bd in enumerate((b_re_d, b_im_d)):
                bv = bd.rearrange("d (G r) p h -> r p (d G) h", r=2)
                for par in range(2):
                    k.dma("sp", Bl[ri_][par * 64:(par + 1) * 64, :, :], bv[par], writes=[R_t])
            for ri_, cd in enumerate((c_re_d, c_im_d)):
                cvw = cd.rearrange("d (G g) h p -> (g h) (d G) p", g=8)
                C4 = Cin[ri_].rearrange("q a (r p) -> q a r p", r=2)
                for dup in range(2):
                    k.dma("sp", C4[:, :, dup, :], cvw, writes=[R_t])
        qrb = Pq[:, 2, :][:, :, None].to_broadcast([128, 64, 16])
        qib = Pq[:, 3, :][:, :, None].to_broadcast([128, 64, 16])
        T1("dve", lambda e: e.tensor_tensor(out=Bt[0], in0=Bl[0], in1=qrb, op=ALU.mult))
        T1("dve", lambda e: e.tensor_tensor(out=Bt[1], in0=Bl[1], in1=qib, op=ALU.mult))
        T1("dve", lambda e: e.tensor_tensor(out=Bb[0], in0=Bt[0], in1=Bt[1], op=ALU.subtract))
        T1("dve", lambda e: e.tensor_tensor(out=Bt[0], in0=Bl[1], in1=qrb, op=ALU.mult))
        T1("dve", lambda e: e.tensor_tensor(out=Bt[1], in0=Bl[0], in1=qib, op=ALU.mult))
        T1("dve", lambda e: e.tensor_tensor(out=Bb[1], in0=Bt[0], in1=Bt[1], op=ALU.add))
        for ri_ in range(2):
            T1("dve", lambda e, ri_=ri_: e.memset(BQ[ri_], 0.0))
            BQ5 = BQ[ri_].rearrange("q a (g r h) -> q (a g) r h", r=2, h=16)
            for par in range(2):
                ps_ = slice(par * 64, (par + 1) * 64)
                T1("dve", lambda e, BQ5=BQ5, par=par, ps_=ps_, ri_=ri_: e.tensor_copy(out=BQ5[ps_, :, par, :], in_=Bb[ri_][ps_, :, :]))
        for ri_ in range(2):
            C4 = Cin[ri_].rearrange("q a (r p) -> q a r p", r=2)
            for par in range(2):
                k.op("dve", lambda e, C4=C4, par=par, ri_=ri_: e.tensor_scalar(
                    out=C4[:, :, par, :], in0=C4[:, :, par, :], scalar1=rowmask[:, 2 * ri_ + par:2 * ri_ + par + 1],
                    scalar2=None, op0=ALU.mult), reads=[R_rowmask], writes=[R_t])
        CT6 = CTp_sb.rearrange("q (a g r) m -> q a g r m", g=4, r=2)
        for a in range(16):
            pt, rp = next_ps()

            def trb(e, a=a, pt=pt):
                ins = None
                for ri_ in range(2):
                    ins = e.transpose(pt[:, ri_ * 128:(ri_ + 1) * 128], BQ[ri_][:, a, :], ident_f[:])
                    ins = e.transpose(pt[:, 256 + ri_ * 128:256 + (ri_ + 1) * 128], Cin[ri_][:, a, :], ident_f[:])
                return ins
            k.op("pe", trb, reads=[R_t, R_ident], writes=[rp])
            k.op("act", lambda e, a=a, pt=pt: e.activation(out=BTp_sb[:, 2 * a:2 * a + 2, :].rearrange("p a b -> p (a b)"),
                                                         in_=pt[:, 0:256], func=AF.Copy), reads=[rp], writes=[R_t])
            for ri_ in range(2):
                k.op("dve", lambda e, a=a, pt=pt, ri_=ri_: e.tensor_copy(
                    out=CT6[:, a, :, ri_, :], in_=pt[:, 256 + ri_ * 128:256 + (ri_ + 1) * 128].rearrange("p (g m) -> p g m", m=32)),
                    reads=[rp], writes=[R_t])
        k.dma("sp", btp_scr, BTp_sb.rearrange("p a b -> p (a b)"), reads=[R_t], writes=[R_scr])
        k.dma("sp", ctp_scr, CTp_sb.rearrange("p a b -> p (a b)"), reads=[R_t], writes=[R_scr])
        k.barrier()

    def s5_branch(T, latent, pidx):
        k.barrier()
        uT = carve(0, [128, 8, 1024], BF16)
        BTp = carve(16384, [128, 32, 128], BF16)
        CTp = carve(24576, [128, 128, 32], BF16)
        bu = carve(32768, [128, 128, TB], F32)
        hist = carve(49152, [128, 128, TB], BF16)
        yacc = carve(65536, [128, 8, 1024], F32)
        R_uT, R_BT, R_CT, R_bu, R_hist, R_yacc = (Reg(n) for n in ("uT", "BTp", "CTp", "bu", "hist", "yacc"))
        if not cfg.get("s5_noscr"):
            k.dma("sp", BTp.rearrange("p a b -> p (a b)"), btp_scr, reads=[R_scr], writes=[R_BT])
            k.dma("sp", CTp.rearrange("p a b -> p (a b)"), ctp_scr, reads=[R_scr], writes=[R_CT])

        def epi_u(pt, rp, cc, tb, N):
            tsl = slice(tb * 512, tb * 512 + N)
            k.op("act", lambda e: e.activation(out=uT[:, cc, tsl], in_=pt[:, 0:N], func=AF.Copy), reads=[rp], writes=[R_uT])
            k.op("dve", lambda e: e.tensor_scalar(out=yacc[:, cc, tsl], in0=pt[:, 0:N], scalar1=dsk[:, cc:cc + 1], scalar2=None,
                                                  op0=ALU.mult), reads=[rp, R_dsk, R_uT], writes=[R_yacc])
        if not cfg.get("s5_nou"):
            linear_fm(w_in_v, 16, C_U, 1024, hT, R_hT, T, epi_u)
        if latent:
            R_h0 = Reg("h0rows")
            h0rows = carve(57344, [64, 2, 128], F32)[0:64, :, :]
            k.dma("sp", h0rows[:, 0, :], h0re_d.rearrange("d (G r) p -> (d G) (r p)", r=2), writes=[R_h0])
            k.dma("sp", h0rows[:, 1, :], h0im_d.rearrange("d (G r) p -> (d G) (r p)", r=2), writes=[R_h0])
            pt, rp = next_ps()

            def trh(e, pt=pt):
                e.transpose(pt[:, 0:64], h0rows[:, 0, :], ident_f[0:64, 0:64])
                return e.transpose(pt[:, 64:128], h0rows[:, 1, :], ident_f[0:64, 0:64])
            k.op("pe", trh, reads=[R_h0, R_ident], writes=[rp])
            k.op("dve", lambda e, pt=pt: e.tensor_copy(out=Xst[:, 0, :], in_=pt[:, 0:128]), reads=[rp], writes=[R_X])
        else:
            k.op("dve", lambda e: e.memset(Xst[:, 0, :], 0.0), writes=[R_X])
        stage = cfg.get("s5_stage", 9)
        if stage <= 1:
            k.barrier()
            return
        bu6 = bu.rearrange("q (r d g w) t -> q r d g w t", r=2, d=2, g=8, w=4)
        AAf = AAt[:].rearrange("p a b -> p (a b)")
        BBf = BBt[:].rearrange("p a b -> p (a b)")
        pp = 0
        nb = T // TB
        for kb in range(nb):
            f0 = kb * TB
            b0 = T - (kb + 1) * TB
            for di in range(2):
                banks = [next_ps() for _ in range(4)]

                def mmbu(e, di=di, banks=banks, f0=f0, b0=b0):
                    ins = None
                    for ri_ in range(2):
                        for G in range(8):
                            j = ri_ * 8 + G
                            for rg in range(cfg.get('s5_nrg', 4)):
                                rs = slice(rg * 32, (rg + 1) * 32)
                                if di == 0:
                                    rhs = uT[rs, G, f0:f0 + TB]
                                else:
                                    rhs = uT[rs, G, b0:b0 + TB][:, ::-1]
                                ins = e.matmul(banks[rg][0][:, j * TB:(j + 1) * TB], lhsT=BTp[rs, (di * 8 + G) * 2 + ri_, :],
                                               rhs=rhs, start=True, stop=True, tile_position=(rg * 32, 0))
                    return ins
                k.op("pe", mmbu, reads=[R_uT, R_BT], writes=[b[1] for b in banks])
                for rg in range(4):
                    eng = "act" if rg % 2 else "dve"
                    src = banks[rg][0][:, 0:16 * TB].rearrange("p (r g t) -> p r g t", r=2, t=TB)
                    dst = bu6[:, :, di, :, rg, :]
                    if eng == "act":
                        k.op("act", lambda e, src=src, dst=dst: e.activation(out=dst, in_=src, func=AF.Copy),
                             reads=[banks[rg][1]], writes=[R_bu])
                    else:
                        k.op("dve", lambda e, src=src, dst=dst: e.tensor_copy(out=dst, in_=src),
                             reads=[banks[rg][1]], writes=[R_bu])
            if stage <= 2:
                continue
            for s_ in range(TB):
                Xc = Xst[:, pp, :]
                Xn = Xst[:, 1 - pp, :]
                Xsw = Xc.rearrange("p (r m) -> p r m", r=2)[:, ::-1, :]
                k.op("dve", lambda e, Xc=Xc: e.tensor_tensor(out=st1[:], in0=Xc, in1=AAf, op=ALU.mult),
                     reads=[R_X, R_AB], writes=[R_st1])
                k.op("dve", lambda e, Xsw=Xsw: e.tensor_tensor(out=st2[:].rearrange("p (r m) -> p r m", r=2), in0=Xsw,
                                                             in1=BBt[:], op=ALU.mult), reads=[R_X, R_AB], writes=[R_st2])
                k.op("dve", lambda e: e.tensor_tensor(out=st1[:], in0=st1[:], in1=st2[:], op=ALU.add),
                     reads=[R_st2], writes=[R_st1])
                k.op("dve", lambda e, Xn=Xn, s_=s_: e.tensor_tensor(out=Xn, in0=st1[:], in1=bu[:, :, s_], op=ALU.add),
                     reads=[R_st1, R_bu], writes=[R_X])
                k.op("act", lambda e, Xn=Xn, s_=s_: e.activation(out=hist[:, :, s_], in_=Xn, func=AF.Copy),
                     reads=[R_X], writes=[R_hist])
                pp = 1 - pp
            if stage <= 3:
                continue
            pty, rpy = next_ps()

            def mmy(e, pty=pty):
                ins = None
                for di in range(2):
                    for G in range(8):
                        for j in range(4):
                            G2 = 4 * G + j
                            for ri_ in range(2):
                                ins = e.matmul(pty[32 * j:32 * j + 32, (di * 8 + G) * TB:(di * 8 + G + 1) * TB],
                                               lhsT=CTp[:, (di * 32 + G2) * 2 + ri_, :], rhs=hist[:, ri_ * 64 + di * 32 + G2, :],
                                               start=(ri_ == 0), stop=(ri_ == 1), tile_position=(0, 32 * j))
                return ins
            k.op("pe", mmy, reads=[R_hist, R_CT], writes=[rpy])
            k.op("dve", lambda e, pty=pty, f0=f0: e.tensor_tensor(
                out=yacc[:, :, f0:f0 + TB], in0=yacc[:, :, f0:f0 + TB],
                in1=pty[:, 0:8 * TB].rearrange("p (g t) -> p g t", t=TB), op=ALU.add), reads=[rpy], writes=[R_yacc])
            k.op("dve", lambda e, pty=pty, b0=b0: e.tensor_tensor(
                out=yacc[:, :, b0:b0 + TB], in0=yacc[:, :, b0:b0 + TB],
                in1=pty[:, 8 * TB:16 * TB].rearrange("p (g t) -> p g t", t=TB)[:, :, ::-1], op=ALU.add),
                reads=[rpy], writes=[R_yacc])
        if not latent:
            pt, rp = next_ps()
            Xf = Xst[:, pp, :]
            k.op("pe", lambda e, pt=pt, Xf=Xf: (e.transpose(pt[0:64, 0:128], Xf[:, 0:64], ident_f[:]),
                                              e.transpose(pt[0:64, 128:256], Xf[:, 64:128], ident_f[:]))[1],
                 reads=[R_X, R_ident], writes=[rp])
            fin = carve(57344, [64, 256], F32)[0:64, :]
            R_fin = Reg("s5fin")
            k.op("dve", lambda e, pt=pt: e.tensor_copy(out=fin, in_=pt[0:64, 0:256]), reads=[rp], writes=[R_fin])
            for ri_ in range(2):
                k.dma("sp", news5[ri_][pidx].rearrange("d (G r) p -> (d G) (r p)", r=2), fin[:, ri_ * 128:(ri_ + 1) * 128],
                      reads=[R_fin], writes=[R_news5])
        if "s5y" in dbg and dbg_cur[0]:
            R_d = Reg("dbgs5y")
            k.dma("sp", dbg_outs["s5y"].rearrange("p (a b) -> p a b", b=T), yacc[:, :, 0:T], reads=[R_yacc], writes=[R_d])
            out_regs.append(R_d)
        gw = carve(0, [128, 8, 1024], F32)
        R_gw = Reg("gw")
        ya = yacc[:, :, 0:T]
        gv = gw[:, :, 0:T]
        k.op("dve", lambda e: e.tensor_tensor(out=gv, in0=ya, in1=ya, op=ALU.mult), reads=[R_yacc, R_uT, R_BT, R_CT, R_hist, R_bu], writes=[R_gw])
        k.op("dve", lambda e: e.tensor_scalar(out=gv, in0=gv, scalar1=0.044715, scalar2=1.0, op0=ALU.mult, op1=ALU.add),
             reads=[R_gw], writes=[R_gw])
        k.op("dve", lambda e: e.tensor_tensor(out=gv, in0=gv, in1=ya, op=ALU.mult), reads=[R_yacc], writes=[R_gw])
        k.op("act", lambda e: e.activation(out=gv, in_=gv, func=AF.Sigmoid, scale=1.5957691216), reads=[R_gw], writes=[R_gw])
        k.op("dve", lambda e: e.tensor_tensor(out=gyT[:, :, 0:T], in0=gv, in1=ya, op=ALU.mult), reads=[R_gw, R_yacc], writes=[R_gyT])
        k.barrier()

    def retention(T, latent, off, pidx):
        nt = T // 128
        R_rope = R_o
        if latent:
            k.dma("sp", ropet.rearrange("p a b -> p (a b)"), rope_d[:], writes=[R_o])

        def epi_qk(dst, R_dst, scale):
            def epi(pt, rp, cc, tb, N):
                h = cc
                tsl = slice(tb * 512, tb * 512 + N)
                if not latent:
                    k.op("act", lambda e: e.activation(out=dst[:, h, tsl], in_=pt[:, 0:N], func=AF.Copy, scale=scale),
                         reads=[rp], writes=[R_dst])
                    return
                k.op("act", lambda e: e.activation(out=rtmp[:, 0:N], in_=pt[:, 0:N], func=AF.Copy, scale=scale),
                     reads=[rp], writes=[R_rtmp])
                p2, rp2 = next_ps()
                k.op("pe", lambda e: e.matmul(p2[:, 0:N], lhsT=psw[:], rhs=rtmp[:, 0:N], start=True, stop=True),
                     reads=[R_rtmp, R_psw], writes=[rp2])
                k.op("dve", lambda e: e.tensor_tensor(out=rt1[:, 0:N], in0=p2[:, 0:N], in1=ropet[:, 1, tsl], op=ALU.mult),
                     reads=[rp2, R_rope], writes=[R_rt1])
                k.op("dve", lambda e: e.tensor_tensor(out=rtmp[:, 0:N], in0=rtmp[:, 0:N], in1=ropet[:, 0, tsl], op=ALU.mult),
                     reads=[R_rope], writes=[R_rtmp])
                k.op("dve", lambda e: e.tensor_tensor(out=dst[:, h, tsl], in0=rtmp[:, 0:N], in1=rt1[:, 0:N], op=ALU.add),
                     reads=[R_rtmp, R_rt1], writes=[R_dst])
            return epi
        linear_fm(w_in_v, 16, C_Q, 1024, hT, R_hT, T, epi_qk(qT, R_qT, DK ** -0.5))
        linear_fm(w_in_v, 16, C_K, 1024, hT, R_hT, T, epi_qk(kT, R_kT, 1.0))

        def epi_v(pt, rp, cb, tt):
            k.op("act", lambda e: e.activation(out=v_tm[:, tt, cb * WB:(cb + 1) * WB], in_=pt[:, 0:WB], func=AF.Copy),
                 reads=[rp], writes=[R_v])
        linear_tm(w_in_v, 16, C_V, 2048, hT, R_hT, T, epi_v)

        for h in cfg.get('ret_head_list', list(range(cfg.get('ret_heads', NH)))):
            for di, (Sb_, R_Sb, order) in enumerate(((Sfb, R_Sfb, list(range(nt))), (Sbb, R_Sbb, list(range(nt - 1, -1, -1))))):
                col = di * 8 + h
                if latent:
                    k.dma("sp", Sst[:], sret_d[di, h, :, :], writes=[R_S])
                else:
                    k.op("dve", lambda e: e.memset(Sst[:], 0.0), writes=[R_S])
                for c in order:
                    k.op("act", lambda e, c=c, Sb_=Sb_: e.activation(out=Sb_[:, c, :], in_=Sst[:], func=AF.Copy),
                         reads=[R_S], writes=[R_Sb])
                    pt, rp = next_ps()
                    k.op("pe", lambda e, c=c, pt=pt: e.matmul(pt[:, 0:128], lhsT=kT[:, h, c * 128:(c + 1) * 128],
                                                             rhs=ident_b[:], start=True, stop=True),
                         reads=[R_kT, R_identb], writes=[rp])
                    k.op("dve", lambda e, pt=pt, col=col: e.tensor_scalar(out=kd[:], in0=pt[:, 0:128],
                                                                         scalar1=dec[:, 1, col:col + 1], scalar2=None,
                                                                         op0=ALU.mult), reads=[rp, R_dec], writes=[R_kd])
                    p2, rp2 = next_ps()
                    k.op("pe", lambda e, c=c, p2=p2: e.matmul(p2[:, 0:DV], lhsT=kd[:], rhs=v_tm[:, c, h * DV:(h + 1) * DV],
                                                             start=True, stop=True), reads=[R_kd, R_v], writes=[rp2])
                    k.op("dve", lambda e, p2=p2, col=col: e.scalar_tensor_tensor(
                        out=Sst[:], in0=Sst[:], scalar=dec[:, 2, col:col + 1], in1=p2[:, 0:DV], op0=ALU.mult, op1=ALU.add),
                        reads=[rp2, R_dec], writes=[R_S])
                if not latent:
                    k.dma("sp", newret[pidx, di, h, :, :], Sst[:], reads=[R_S], writes=[out_regs_ret])
            for c in range(nt):
                csl = slice(c * 128, (c + 1) * 128)
                pt, rp = next_ps()
                k.op("pe", lambda e, pt=pt, csl=csl: e.matmul(pt[:, 0:128], lhsT=kT[:, h, csl], rhs=qT[:, h, csl],
                                                             start=True, stop=True), reads=[R_kT, R_qT], writes=[rp])
                k.op("dve", lambda e, pt=pt: e.tensor_tensor(out=Amat[:], in0=pt[:, 0:128], in1=MT[:, h, :], op=ALU.mult),
                     reads=[rp, R_MT], writes=[R_A])
                p2, rp2 = next_ps()

                def mm2(e, p2=p2, c=c, csl=csl):
                    e.matmul(p2[:, 0:DV], lhsT=Amat[:], rhs=v_tm[:, c, h * DV:(h + 1) * DV], start=True, stop=True)
                    return e.matmul(p2[:, DV:2 * DV], lhsT=qT[:, h, csl], rhs=Sfb[:, c, :], start=True, stop=True)
                k.op("pe", mm2, reads=[R_A, R_v, R_qT, R_Sfb], writes=[rp2])
                p3, rp3 = next_ps()
                k.op("pe", lambda e, p3=p3, c=c, csl=csl: e.matmul(p3[:, 0:DV], lhsT=qT[:, h, csl], rhs=Sbb[:, c, :],
                                                                  start=True, stop=True), reads=[R_qT, R_Sbb], writes=[rp3])
                k.op("dve", lambda e, p2=p2: e.tensor_scalar(out=ot[:], in0=p2[:, DV:2 * DV], scalar1=dec[:, 0, h:h + 1],
                                                             scalar2=None, op0=ALU.mult), reads=[rp2, R_dec], writes=[R_ot])
                k.op("dve", lambda e, p3=p3: e.scalar_tensor_tensor(out=ot2[:], in0=p3[:, 0:DV], scalar=dec[:, 0, 8 + h:9 + h],
                                                                    in1=ot[:], op0=ALU.mult, op1=ALU.add),
                     reads=[rp3, R_dec, R_ot], writes=[R_ot2])
                k.op("dve", lambda e, p2=p2: e.tensor_tensor(out=ot[:], in0=p2[:, 0:DV], in1=ot2[:], op=ALU.add),
                     reads=[rp2, R_ot2], writes=[R_ot])
                k.op("act", lambda e: e.activation(out=ot2[:], in_=ot[:], func=AF.Identity, accum_out=gst[:, 0:1]),
                     reads=[R_ot], writes=[R_ot2, R_gst])
                k.op("dve", lambda e: e.tensor_scalar(out=gst[:, 1:2], in0=gst[:, 0:1], scalar1=-1.0 / DV, scalar2=None,
                                                      op0=ALU.mult), reads=[R_gst], writes=[R_gst])
                k.op("dve", lambda e: e.tensor_scalar(out=ot[:], in0=ot[:], scalar1=gst[:, 1:2], scalar2=None,
                                                      op0=ALU.add), reads=[R_gst], writes=[R_ot])
                k.op("act", lambda e: e.activation(out=ot2[:], in_=ot[:], func=AF.Square, accum_out=gst[:, 2:3]),
                     reads=[R_ot], writes=[R_ot2, R_gst])
                k.op("dve", lambda e: e.tensor_scalar(out=gst[:, 3:4], in0=gst[:, 2:3], scalar1=1.0 / DV, scalar2=EPS,
                                                      op0=ALU.mult, op1=ALU.add), reads=[R_gst], writes=[R_gst])
                k.op("act", lambda e: e.activation(out=gst[:, 4:5], in_=gst[:, 3:4], func=AF.Sqrt),
                     reads=[R_gst], writes=[R_gst])
                k.op("dve", lambda e: e.reciprocal(out=gst[:, 5:6], in_=gst[:, 4:5]), reads=[R_gst], writes=[R_gst])
                k.op("dve", lambda e, c=c: e.scalar_tensor_tensor(out=o_tm[:, c, h * DV:(h + 1) * DV], in0=ot[:],
                                                                  scalar=gst[:, 5:6], in1=gnw[:, h * DV:(h + 1) * DV],
                                                                  op0=ALU.mult, op1=ALU.mult),
                     reads=[R_ot, R_gst, R_gnw], writes=[R_o])

        def epi_g(pt, rp, cb, tt):
            k.op("act", lambda e: e.activation(out=sg[:, 0:WB], in_=pt[:, 0:WB], func=AF.Silu), reads=[rp], writes=[R_sg])
            k.op("dve", lambda e: e.tensor_tensor(out=o_tm[:, tt, cb * WB:(cb + 1) * WB],
                                                  in0=o_tm[:, tt, cb * WB:(cb + 1) * WB], in1=sg[:, 0:WB], op=ALU.mult),
                 reads=[R_sg], writes=[R_o])
        linear_tm(w_in_v, 16, C_G, 2048, hT, R_hT, T, epi_g)

    w_glu_v = din("w_s5_glu", [S5W, 2 * D]).rearrange("(kc p) f -> p kc f", p=128)
    w_ro_v = din("w_ret_out", [D, D]).rearrange("(kc p) f -> p kc f", p=128)
    w_out_v = din("w_out", [D, D]).rearrange("(kc p) f -> p kc f", p=128)
    w_router_d = din("w_router", [D, NE])
    moec_d = din("moe_consts", [128, 384])
    x2_scr = nc.dram_tensor("x2_scr", [NTOK, D], F32).ap()
    h2_scr = nc.dram_tensor("h2_scr", [NTOK, D], BF16).ap()
    R_x2 = Reg("x2scr")
    R_h2s = Reg("h2scr")
    NGT = NTOK // 128
    aff_all = sb("aff_all", [128, NGT, NE])
    affhl = sb("affhl", [128, NGT, NE, 2], BF16)
    posm_all = sb("posm_all", [128, NGT, NE])
    sel_all = sb("sel_all", [128, NGT, NE])
    R_sel = Reg("sel")
    R_aff = Reg("aff")
    R_posm = Reg("posm")
    moec = sb("moec", [128, 384])
    R_moec = Reg("moec")
    k.dma("sp", moec[:], moec_d[:], writes=[R_moec])
    iota_row = moec[:, 0:128]
    onesUb = sb("onesUb", [128, 2, 128], BF16)
    R_oUb = Reg("onesUb")
    k.op("dve", lambda e: e.tensor_copy(out=onesUb[:].rearrange("p a b -> p (a b)"), in_=moec[:, 128:384]), reads=[R_moec], writes=[R_oUb])
    wr = sb("wr", [128, 16, NE], BF16)
    R_wr = Reg("wr")
    k.dma("pool", wr[:], w_router_d.rearrange("(kc p) e -> p kc e", p=128), writes=[R_wr])
    sm = sb("sm", [128, 8])
    R_sm = Reg("sm")
    selt = sb("selt", [128, NE], BF16)
    R_selt = Reg("selt")

    def lin_chunk(wview, KC, col, rhsT, R_rhs, T, epi):
        wt, rw = next_w()
        wv = wt[:, 0:KC * 128].rearrange("p (kc f) -> p kc f", f=128)
        k.dma("pool", wv, wview[:, :, col:col + 128], writes=[rw])
        for tb in range((T + 511) // 512):
            N = min(512, T - tb * 512)
            pt, rp = next_ps()

            def mm(e, wv=wv, pt=pt, tb=tb, N=N):
                ins = None
                for kc in range(KC):
                    ins = e.matmul(pt[:, 0:N], lhsT=wv[:, kc, :], rhs=rhsT[:, kc, tb * 512: tb * 512 + N],
                                   start=(kc == 0), stop=(kc == KC - 1))
                return ins
            k.op("pe", mm, reads=[rw, R_rhs], writes=[rp])
            epi(pt, rp, slice(tb * 512, tb * 512 + N), N)

    def merge_out(T, cv, off):
        nt = T // 128
        k.barrier()
        oT = carve(0, [128, 16, 1024], BF16)
        mergedT = carve(32768, [128, 16, 1024], BF16)
        R_oT = Reg("oT")
        R_mT = Reg("mergedT")
        for m in range(16):
            for tg in range((nt + 3) // 4):
                tiles = list(range(tg * 4, min(nt, tg * 4 + 4)))
                pt, rp = next_ps()

                def tr(e, m=m, tiles=tiles, pt=pt):
                    ins = None
                    for j, tt in enumerate(tiles):
                        ins = e.matmul(pt[:, j * 128:(j + 1) * 128], lhsT=o_tm[:, tt, m * 128:(m + 1) * 128], rhs=ident_b[:],
                                       start=True, stop=True)
                    return ins
                k.op("pe", tr, reads=[R_o, R_identb], writes=[rp])
                n_ = len(tiles) * 128
                eng = "act" if (m + tg) % 2 else "dve"
                if eng == "act":
                    k.op("act", lambda e, m=m, tg=tg, n_=n_, pt=pt: e.activation(out=oT[:, m, tg * 512: tg * 512 + n_], in_=pt[:, 0:n_],
                                                                               func=AF.Copy), reads=[rp], writes=[R_oT])
                else:
                    k.op("dve", lambda e, m=m, tg=tg, n_=n_, pt=pt: e.tensor_copy(out=oT[:, m, tg * 512: tg * 512 + n_], in_=pt[:, 0:n_]),
                         reads=[rp], writes=[R_oT])
        k.barrier()
        tA = carve(65536, [128, 1024], F32)
        tB = carve(69632, [128, 1024], F32)
        g1row = carve(73728, [128, D], F32)
        xb = [carve(81920 + i_ * 1024, [128, 256], F32) for i_ in range(2)]
        x2b = [carve(83968 + i_ * 1024, [128, 256], F32) for i_ in range(2)]
        gbm = carve(86016, [128, 128], F32)
        R_tA, R_tB, R_g1, R_gbm = Reg("tA"), Reg("tB"), Reg("g1row"), Reg("gbm")
        R_xb = [Reg("xb0"), Reg("xb1")]
        R_x2b = [Reg("x2b0"), Reg("x2b1")]
        for m in range(16):
            def e1(pt, rp, tsl, N):
                k.op("act", lambda e: e.activation(out=tA[:, tsl], in_=pt[:, 0:N], func=AF.Sigmoid), reads=[rp], writes=[R_tA])
            lin_chunk(w_glu_v, 8, D + m * 128, gyT, R_gyT, T, e1)

            def e2(pt, rp, tsl, N):
                k.op("dve", lambda e: e.tensor_tensor(out=tA[:, tsl], in0=tA[:, tsl], in1=pt[:, 0:N], op=ALU.mult), reads=[rp], writes=[R_tA])
            lin_chunk(w_glu_v, 8, m * 128, gyT, R_gyT, T, e2)

            def e3(pt, rp, tsl, N):
                k.op("act", lambda e: e.activation(out=tB[:, tsl], in_=pt[:, 0:N], func=AF.Sigmoid), reads=[rp], writes=[R_tB])
                k.op("dve", lambda e: e.tensor_tensor(out=tA[:, tsl], in0=tA[:, tsl], in1=tB[:, tsl], op=ALU.mult), reads=[R_tB], writes=[R_tA])
            lin_chunk(w_in_v, 16, C_GA + m * 128, hT, R_hT, T, e3)

            def e4(pt, rp, tsl, N):
                k.op("act", lambda e: e.activation(out=tB[:, tsl], in_=pt[:, 0:N], func=AF.Sigmoid), reads=[rp], writes=[R_tB])
            lin_chunk(w_in_v, 16, C_GB + m * 128, hT, R_hT, T, e4)

            def e5(pt, rp, tsl, N, m=m):
                k.op("dve", lambda e: e.tensor_tensor(out=tB[:, tsl], in0=tB[:, tsl], in1=pt[:, 0:N], op=ALU.mult), reads=[rp], writes=[R_tB])
                k.op("dve", lambda e: e.tensor_tensor(out=mergedT[:, m, tsl], in0=tA[:, tsl], in1=tB[:, tsl], op=ALU.add),
                     reads=[R_tA, R_tB], writes=[R_mT])
            lin_chunk(w_ro_v, 16, m * 128, oT, R_oT, T, e5)
        bcast_row(32, cv, g1row, R_g1, gbm, R_gbm)

        def epi_o(pt, rp, cb, tt):
            i_ = (cb + tt) % 2
            cols = slice(cb * WB, (cb + 1) * WB)
            rows = slice(off + tt * 128, off + (tt + 1) * 128)
            k.dma("sp", xb[i_], xin[rows, cols], writes=[R_xb[i_]])
            k.op("dve", lambda e: e.tensor_tensor(out=x2b[i_], in0=pt[:, 0:WB], in1=g1row[:, cols], op=ALU.mult),
                 reads=[rp, R_g1], writes=[R_x2b[i_]])
            k.op("dve", lambda e: e.tensor_tensor(out=x2b[i_], in0=x2b[i_], in1=xb[i_], op=ALU.add), reads=[R_xb[i_]], writes=[R_x2b[i_]])
            k.dma("sp", x2_scr[rows, cols], x2b[i_], reads=[R_x2b[i_]], writes=[R_x2])
        linear_tm(w_out_v, 16, 0, D, mergedT, R_mT, T, epi_o)
        k.barrier()

    def bcast_row(sidx, cv, dst, R_dst, gbm, R_gbm):
        for q4 in range(4):
            pt, rp = next_ps()
            for j in range(4):
                m = q4 * 4 + j
                k.op("dve", lambda e, m=m: e.tensor_copy(out=gbm, in_=mod[:, sidx + m, cv:cv + 1].to_broadcast([128, 128])),
                     reads=[R_mod], writes=[R_gbm])
                k.op("pe", lambda e, j=j, pt=pt: e.matmul(pt[:, j * 128:(j + 1) * 128], lhsT=gbm, rhs=ident_f[:], start=True, stop=True),
                     reads=[R_gbm, R_ident], writes=[rp])
            k.op("act", lambda e, q4=q4, pt=pt: e.activation(out=dst[:, q4 * 512:(q4 + 1) * 512], in_=pt[:, :], func=AF.Copy),
                 reads=[rp], writes=[R_dst])

    def norm2_router(T, cv, off, cap):
        nt = T // 128
        gt0 = off // 128
        norm_to_T(lambda tt: x2_scr[off + tt * 128: off + (tt + 1) * 128, :], nt, 1, 3, cv, hT, R_hT, R_src_fn=R_x2)
        k.barrier()
        h2t = [carve(0 + i_ * 4096, [128, D], BF16) for i_ in range(2)]
        R_h2t = [Reg("h2t0"), Reg("h2t1")]
        affT = carve(8192, [16, 1024], F32)[0:16, :]
        R_affT = Reg("affT")
        bcs = carve(12288, [128, 1024], F32)
        R_bcs = Reg("bcs")
        cmpj = carve(16384, [128, 1024], BF16)
        dj = carve(18432, [128, 128], F32)
        R_cmpj = Reg("cmpj")
        for tt in range(nt):
            gt = gt0 + tt
            i_ = tt % 2
            tsl = slice(tt * 128, (tt + 1) * 128)
            for q4 in range(4):
                pt, rp = next_ps()

                def trb(e, q4=q4, pt=pt, tsl=tsl):
                    ins = None
                    for j in range(4):
                        ins = e.matmul(pt[:, j * 128:(j + 1) * 128], lhsT=hT[:, q4 * 4 + j, tsl], rhs=ident_b[:], start=True, stop=True)
                    return ins
                k.op("pe", trb, reads=[R_hT, R_identb], writes=[rp])
                k.op("act", lambda e, q4=q4, pt=pt, i_=i_: e.activation(out=h2t[i_][:, q4 * 512:(q4 + 1) * 512], in_=pt[:, :], func=AF.Copy),
                     reads=[rp], writes=[R_h2t[i_]])
            k.dma("sp", h2_scr[off + tt * 128: off + (tt + 1) * 128, :], h2t[i_], reads=[R_h2t[i_]], writes=[R_h2s])
            pt, rp = next_ps()

            def mmr(e, pt=pt, tsl=tsl):
                ins = None
                for kc in range(16):
                    ins = e.matmul(pt[:, 0:NE], lhsT=hT[:, kc, tsl], rhs=wr[:, kc, :], start=(kc == 0), stop=(kc == 15))
                return ins
            k.op("pe", mmr, reads=[R_hT, R_wr], writes=[rp])
            k.op("dve", lambda e, pt=pt: e.tensor_reduce(out=sm[:, 0:1], in_=pt[:, 0:NE], op=ALU.max, axis=AX.X), reads=[rp], writes=[R_sm])
            k.op("dve", lambda e: e.tensor_scalar(out=sm[:, 1:2], in0=sm[:, 0:1], scalar1=-1.0, scalar2=None, op0=ALU.mult),
                 reads=[R_sm], writes=[R_sm])
            k.op("act", lambda e, pt=pt, gt=gt: e.activation(out=aff_all[:, gt, :], in_=pt[:, 0:NE], func=AF.Exp, bias=sm[:, 1:2],
                                                            accum_out=sm[:, 2:3]), reads=[rp, R_sm], writes=[R_aff, R_sm])
            k.op("dve", lambda e: e.reciprocal(out=sm[:, 3:4], in_=sm[:, 2:3]), reads=[R_sm], writes=[R_sm])
            k.op("dve", lambda e, gt=gt: e.tensor_scalar(out=aff_all[:, gt, :], in0=aff_all[:, gt, :], scalar1=sm[:, 3:4], scalar2=None,
                                                         op0=ALU.mult), reads=[R_sm], writes=[R_aff])
            k.op("dve", lambda e, gt=gt: e.tensor_copy(out=affhl[:, gt, :, 0], in_=aff_all[:, gt, :]), reads=[R_aff], writes=[R_aff])
            k.op("dve", lambda e, gt=gt: e.tensor_tensor(out=affhl[:, gt, :, 1], in0=aff_all[:, gt, :], in1=affhl[:, gt, :, 0],
                                                         op=ALU.subtract), reads=[R_aff], writes=[R_aff])
            pt2, rp2 = next_ps()
            k.op("pe", lambda e, pt2=pt2, gt=gt: e.transpose(pt2[0:16, 0:128], aff_all[:, gt, :], ident_f[:]),
                 reads=[R_aff, R_ident], writes=[rp2])
            k.op("dve", lambda e, pt2=pt2, tsl=tsl: e.tensor_copy(out=affT[:, tsl], in_=pt2[0:16, 0:128]), reads=[rp2], writes=[R_affT])
        oh = carve(20992, [128, 128], F32)[0:16, :]
        R_oh = Reg("oh")
        for ex in range(NE):
            k.op("dve", lambda e, ex=ex: e.tensor_copy(out=oh, in_=ident_f[0:16, ex:ex + 1].to_broadcast([16, 128])),
                 reads=[R_ident], writes=[R_oh])
            for tb in range((T + 511) // 512):
                N = min(512, T - tb * 512)
                pt, rp = next_ps()
                k.op("pe", lambda e, pt=pt, tb=tb, N=N, ex=ex: e.matmul(pt[:, 0:N], lhsT=oh, rhs=affT[:, tb * 512: tb * 512 + N],
                                                                      start=True, stop=True), reads=[R_affT, R_oh], writes=[rp])
                k.op("act", lambda e, pt=pt, tb=tb, N=N: e.activation(out=bcs[:, tb * 512: tb * 512 + N], in_=pt[:, 0:N], func=AF.Copy),
                     reads=[rp], writes=[R_bcs])
            for tt in range(nt):
                gt = gt0 + tt
                tsl = slice(tt * 128, (tt + 1) * 128)
                k.op("dve", lambda e, tsl=tsl: e.scalar_tensor_tensor(out=dj, in0=bcs[:, tsl], scalar=1.0, in1=ident_f[:], op0=ALU.mult,
                                                                     op1=ALU.mult, accum_out=sm[:, 4:5]),
                     reads=[R_bcs, R_ident], writes=[R_cmpj, R_sm])
                k.op("dve", lambda e: e.tensor_scalar(out=cmpj[:, 0:T], in0=bcs[:, 0:T], scalar1=sm[:, 4:5], scalar2=None, op0=ALU.is_gt,
                                                      op1=ALU.add, accum_out=sm[:, 5:6]), reads=[R_bcs, R_sm], writes=[R_cmpj, R_sm])
                k.op("dve", lambda e, gt=gt, ex=ex: e.tensor_scalar(out=sel_all[:, gt, ex:ex + 1], in0=sm[:, 5:6], scalar1=float(cap) - 0.5,
                                                                    scalar2=None, op0=ALU.is_lt), reads=[R_sm], writes=[R_sel])
        for tt in range(nt):
            gt = gt0 + tt
            pt, rp = next_ps()
            for t2 in range(tt + 1):
                k.op("dve", lambda e, t2=t2: e.tensor_copy(out=selt[:], in_=sel_all[:, gt0 + t2, :]), reads=[R_sel], writes=[R_selt])
                k.op("pe", lambda e, pt=pt, t2=t2, tt=tt: e.matmul(pt[:, 0:NE], lhsT=onesUb[:, 0 if t2 < tt else 1, :], rhs=selt[:],
                                                                  start=(t2 == 0), stop=(t2 == tt)), reads=[R_selt, R_oUb], writes=[rp])
            pos1 = carve(20480, [128, NE], F32)
            k.op("dve", lambda e, pt=pt, pos1=pos1: e.tensor_scalar(out=pos1, in0=pt[:, 0:NE], scalar1=1.0, scalar2=None, op0=ALU.add),
                 reads=[rp], writes=[R_cmpj])
            k.op("dve", lambda e, gt=gt, pos1=pos1: e.tensor_tensor(out=pos1, in0=pos1, in1=sel_all[:, gt, :], op=ALU.mult),
                 reads=[R_sel], writes=[R_cmpj])
            k.op("dve", lambda e, gt=gt, pos1=pos1: e.tensor_scalar(out=posm_all[:, gt, :], in0=pos1, scalar1=-1.0, scalar2=None, op0=ALU.add),
                 reads=[R_cmpj], writes=[R_posm])
        k.barrier()

    wg_d = din("w_exp_gate", [NEXP, D, FF])
    wu_d = din("w_exp_up", [NEXP, D, FF])
    wd_d = din("w_exp_down", [NEXP, FF, D])
    caps = [2 * r_[0] // NE for r_ in reqs]
    sos = [sum(caps[:i_]) for i_ in range(len(reqs))]
    NSLOT = sum(caps)
    eo_scr = nc.dram_tensor("eo_scr", [NE, NSLOT, D], BF16).ap()
    R_eo = Reg("eo_scr")
    mtiles = []
    cur = [0, 0]
    for r_i in range(len(reqs)):
        if cur[1] + caps[r_i] > 128:
            mtiles.append(tuple(cur))
            cur = [sos[r_i], 0]
        cur[1] += caps[r_i]
    mtiles.append(tuple(cur))

    def moe():
        k.barrier()
        h2tm = carve(0, [128, NGT, D], BF16)
        R_h2tm = Reg("h2tm")
        k.dma("sp", h2tm, h2_scr.rearrange("(t p) f -> p t f", p=128), reads=[R_h2s], writes=[R_h2tm])
        xsT = carve(49152, [128, 16, NSLOT], BF16)
        hidT = carve(55296, [128, 32, NSLOT], BF16)
        wdb = [carve(67584 + i_ * 8192, [128, 32, 128], BF16) for i_ in range(2)]
        Sx = carve(83968, [128, NGT, 128], BF16)
        gsl = carve(87040, [128, 4], F32)
        sgt = carve(87104, [128, NSLOT], F32)
        eot = carve(88000, [128, 128], F32)
        R_xsT, R_hidT, R_Sx, R_gsl, R_sgt, R_eot = (Reg(n) for n in ("xsT", "hidT", "Sx", "gsl", "sgt", "eot"))
        R_wdb = [Reg("wdb0"), Reg("wdb1")]
        eoh = hT[:].rearrange("p a b -> p (a b)")[:, 0:len(mtiles) * D].rearrange("p (a b) -> p a b", b=D)
        R_eoh = R_hT
        wdi = 0
        gyflat = gyT[:].rearrange("p a b -> p (a b)")
        gu_slots = [(wslots[0][:], R_w[0]), (wslots[1][:], R_w[1]), (gyflat[:, 0:4096], Reg("gus2")), (gyflat[:, 4096:8192], Reg("gus3"))]
        gui = 0
        wdb.append(xts[0][:].bitcast(BF16).rearrange("p (a b) -> p a b", b=128))
        R_wdb.append(Reg("wdb2"))
        for ex in range(NEXP):
            for r_i, (T, latent, cv, off) in enumerate(reqs):
                cap = caps[r_i]
                for tt in range(T // 128):
                    gt = off // 128 + tt
                    k.op("dve", lambda e, gt=gt, cap=cap, ex=ex: e.tensor_scalar(out=Sx[:, gt, 0:cap], in0=iota_row[:, 0:cap],
                                                                               scalar1=posm_all[:, gt, ex:ex + 1], scalar2=None,
                                                                               op0=ALU.is_equal), reads=[R_posm, R_moec], writes=[R_Sx])
            for r_i, (T, latent, cv, off) in enumerate(reqs):
                cap = caps[r_i]
                nt = T // 128
                gt0 = off // 128
                for k4 in range(4):
                    pt, rp = next_ps()

                    def mg(e, pt=pt, k4=k4, cap=cap, nt=nt, gt0=gt0):
                        ins = None
                        for j in range(4):
                            kc = k4 * 4 + j
                            for tt in range(nt):
                                ins = e.matmul(pt[:, j * cap:(j + 1) * cap], lhsT=h2tm[:, gt0 + tt, kc * 128:(kc + 1) * 128],
                                               rhs=Sx[:, gt0 + tt, 0:cap], start=(tt == 0), stop=(tt == nt - 1))
                        return ins
                    k.op("pe", mg, reads=[R_h2tm, R_Sx], writes=[rp])
                    k.op("act", lambda e, pt=pt, k4=k4, cap=cap, r_i=r_i: e.activation(
                        out=xsT[:, k4 * 4:(k4 + 1) * 4, sos[r_i]:sos[r_i] + cap], in_=pt[:, 0:4 * cap].rearrange("p (a b) -> p a b", b=cap),
                        func=AF.Copy), reads=[rp], writes=[R_xsT])
                mi = [i_ for i_, (s0, n_) in enumerate(mtiles) if s0 <= sos[r_i] < s0 + n_][0]
                po = sos[r_i] - mtiles[mi][0]
                pt, rp = next_ps()

                def mgs(e, pt=pt, cap=cap, nt=nt, gt0=gt0, po=po, ex=ex):
                    ins = None
                    for tt in range(nt):
                        ins = e.matmul(pt[po:po + cap, 0:2], lhsT=Sx[:, gt0 + tt, 0:cap], rhs=affhl[:, gt0 + tt, ex, :],
                                       start=(tt == 0), stop=(tt == nt - 1), tile_position=(0, po))
                    return ins
                k.op("pe", mgs, reads=[R_Sx, R_aff], writes=[rp])
                k.op("dve", lambda e, pt=pt, po=po, cap=cap, mi=mi: e.tensor_reduce(out=gsl[po:po + cap, mi:mi + 1], in_=pt[po:po + cap, 0:2],
                                                                                 op=ALU.add, axis=AX.X),
                     reads=[rp], writes=[R_gsl])
            for fc2 in range(FF // 256):
                wtg, rwg = gu_slots[gui]
                wtu, rwu = gu_slots[gui + 1]
                gui = (gui + 2) % 4
                wvg = wtg.rearrange("p (kc f) -> p kc f", f=256)
                wvu = wtu.rearrange("p (kc f) -> p kc f", f=256)
                k.dma("pool", wvg, wg_d[ex].rearrange("(kc p) f -> p kc f", p=128)[:, :, fc2 * 256:(fc2 + 1) * 256], writes=[rwg])
                k.dma("pool", wvu, wu_d[ex].rearrange("(kc p) f -> p kc f", p=128)[:, :, fc2 * 256:(fc2 + 1) * 256], writes=[rwu])
                for j in range(2):
                    fc = fc2 * 2 + j
                    pg, rpg = next_ps()
                    pu, rpu = next_ps()

                    def mgu(e, wvg=wvg, wvu=wvu, pg=pg, pu=pu, j=j):
                        ins = None
                        for wv_, pp_ in ((wvg, pg), (wvu, pu)):
                            for kc in range(16):
                                ins = e.matmul(pp_[:, 0:NSLOT], lhsT=wv_[:, kc, j * 128:(j + 1) * 128], rhs=xsT[:, kc, :],
                                               start=(kc == 0), stop=(kc == 15))
                        return ins
                    k.op("pe", mgu, reads=[rwg, rwu, R_xsT], writes=[rpg, rpu])
                    k.op("act", lambda e, pg=pg: e.activation(out=sgt, in_=pg[:, 0:NSLOT], func=AF.Silu), reads=[rpg], writes=[R_sgt])
                    k.op("dve", lambda e, pu=pu, fc=fc: e.tensor_tensor(out=hidT[:, fc, :], in0=sgt, in1=pu[:, 0:NSLOT], op=ALU.mult),
                         reads=[rpu, R_sgt], writes=[R_hidT])
            for cb in range(D // 256):
                halves = []
                for hf in range(2):
                    wd_, rwd = wdb[wdi], R_wdb[wdi]
                    wdi = (wdi + 1) % 3
                    wdv = wd_.rearrange("p a b -> p (a b)").rearrange("p (fc f) -> p fc f", f=256)
                    k.dma("pool", wdv, wd_d[ex].rearrange("(fc p) d -> p fc d", p=128)[:, hf * 16:(hf + 1) * 16, cb * 256:(cb + 1) * 256],
                          writes=[rwd])
                    halves.append((wdv, rwd))
                for mi, (s0, n_) in enumerate(mtiles):
                    pt, rp = next_ps()

                    def mdn(e, pt=pt, halves=halves, s0=s0, n_=n_):
                        ins = None
                        for hf in range(2):
                            for f_ in range(16):
                                fc = hf * 16 + f_
                                ins = e.matmul(pt[0:n_, 0:256], lhsT=hidT[:, fc, s0:s0 + n_], rhs=halves[hf][0][:, f_, :], start=(fc == 0),
                                               stop=(fc == 31))
                        return ins
                    k.op("pe", mdn, reads=[halves[0][1], halves[1][1], R_hidT], writes=[rp])
                    k.op("dve", lambda e, pt=pt, n_=n_, mi=mi, cb=cb: e.tensor_scalar(out=eoh[0:n_, mi, cb * 256:(cb + 1) * 256], in0=pt[0:n_, 0:256],
                                                                                   scalar1=gsl[0:n_, mi:mi + 1], scalar2=None, op0=ALU.mult),
                         reads=[rp, R_gsl], writes=[R_eoh])
            for mi, (s0, n_) in enumerate(mtiles):
                k.dma("sp", eo_scr[ex, s0:s0 + n_, :], eoh[0:n_, mi, :], reads=[R_eoh], writes=[R_eo])
        k.barrier()

    def combine(r_i):
        T, latent, cv, off = reqs[r_i]
        cap = caps[r_i]
        nt = T // 128
        gt0 = off // 128
        k.barrier()
        Eo = carve(0, [128, NE, D], BF16)
        STe = carve(65536, [128, NE, 128], BF16)
        Sx1 = carve(69632, [128, 128], BF16)
        yt = carve(69888, [128, D], F32)
        g2row = carve(78080, [128, D], F32)
        x2t = carve(86272, [128, D], F32)
        gbm = carve(94464, [128, 128], F32)
        R_Eo, R_STe, R_Sx1, R_yt, R_g2, R_x2t, R_gbm = (Reg(n) for n in ("Eo", "STe", "Sx1", "yt", "g2row", "x2t", "gbm2"))
        with nc.allow_non_contiguous_dma(reason="per-expert row blocks"):
            k.dma("sp", Eo[0:cap, 0:NEXP, :], eo_scr[0:NEXP, sos[r_i]:sos[r_i] + cap, :].rearrange("e s d -> s e d"), reads=[R_eo], writes=[R_Eo])
        bcast_row(80, cv, g2row, R_g2, gbm, R_gbm)
        for tt in range(nt):
            gt = gt0 + tt
            rows = slice(off + tt * 128, off + (tt + 1) * 128)
            k.dma("sp", x2t, x2_scr[rows, :], reads=[R_x2], writes=[R_x2t])
            for ex in range(NEXP):
                k.op("dve", lambda e, gt=gt, ex=ex: e.tensor_scalar(out=Sx1[:, 0:cap], in0=iota_row[:, 0:cap], scalar1=posm_all[:, gt, ex:ex + 1],
                                                                    scalar2=None, op0=ALU.is_equal), reads=[R_posm, R_moec], writes=[R_Sx1])
                pt, rp = next_ps()
                k.op("pe", lambda e, pt=pt: e.matmul(pt[0:cap, 0:128], lhsT=Sx1[:, 0:cap], rhs=ident_b[:], start=True, stop=True),
                     reads=[R_Sx1, R_identb], writes=[rp])
                k.op("act", lambda e, pt=pt, ex=ex: e.activation(out=STe[0:cap, ex, :], in_=pt[0:cap, 0:128], func=AF.Copy),
                     reads=[rp], writes=[R_STe])
            for cbk in range(4):
                cols = slice(cbk * 512, (cbk + 1) * 512)
                pt, rp = next_ps()

                def msc(e, pt=pt, cols=cols):
                    ins = None
                    for ex in range(NEXP):
                        ins = e.matmul(pt[:, :], lhsT=STe[0:cap, ex, :], rhs=Eo[0:cap, ex, cols], start=(ex == 0), stop=(ex == NEXP - 1))
                    return ins
                k.op("pe", msc, reads=[R_STe, R_Eo], writes=[rp])
                k.op("dve", lambda e, pt=pt, cols=cols: e.tensor_tensor(out=yt[:, cols], in0=pt[:, :], in1=g2row[:, cols], op=ALU.mult),
                     reads=[rp, R_g2], writes=[R_yt])
            k.op("dve", lambda e: e.tensor_tensor(out=yt, in0=yt, in1=x2t, op=ALU.add), reads=[R_x2t], writes=[R_yt])
            k.op("act", lambda e: e.activation(out=x2t, in_=yt, func=AF.Square, accum_out=stat[:, 0:1]), reads=[R_yt], writes=[R_x2t, R_stat])
            k.op("dve", lambda e: e.tensor_scalar(out=stat[:, 1:2], in0=stat[:, 0:1], scalar1=1.0 / D, scalar2=EPS, op0=ALU.mult, op1=ALU.add),
                 reads=[R_stat], writes=[R_stat])
            k.op("act", lambda e: e.activation(out=stat[:, 2:3], in_=stat[:, 1:2], func=AF.Sqrt), reads=[R_stat], writes=[R_stat])
            k.op("dve", lambda e: e.reciprocal(out=stat[:, 3:4], in_=stat[:, 2:3]), reads=[R_stat], writes=[R_stat])
            k.op("dve", lambda e: e.scalar_tensor_tensor(out=yt, in0=yt, scalar=stat[:, 3:4], in1=gnw[:], op0=ALU.mult, op1=ALU.mult),
                 reads=[R_stat, R_gnw], writes=[R_yt])
            k.dma("sp", yout[rows, :], yt, reads=[R_yt], writes=[R_yout])

    R_yout = Reg("yout")
    dbg_cur = [False]
    if not cfg.get('skip_s5'):
        s5_precompute()
    pidx = 0
    for ri, (T, latent, cv, off) in enumerate(reqs):
        nt = T // 128
        k.barrier()
        norm_to_T(lambda tt, off=off: xin[off + tt * 128: off + (tt + 1) * 128, :], nt, 0, 0, cv, hT, R_hT)
        k.barrier()
        dbg_cur[0] = (ri == dbg_req)
        if not cfg.get("skip_s5") and cfg.get("s5_mode") != "pre":
            s5_branch(T, latent, pidx)
        if cfg.get("skip_ret"):
            continue
        retention(T, latent, off, pidx)
        if not latent:
            pidx += 1
        if cfg.get("stop") == "ret":
            continue
        merge_out(T, cv, off)
        norm2_router(T, cv, off, 2 * T // NE)
        if "o_tm" in dbg and ri == dbg_req:
            R_d = Reg("dbgo")
            k.dma("sp", dbg_outs["o_tm"].rearrange("(t p) f -> p t f", p=128), o_tm[:, 0:nt, :], reads=[R_o], writes=[R_d])
            out_regs.append(R_d)
    out_regs.append(out_regs_ret)
    if not cfg.get("skip_s5"):
        out_regs.append(R_news5)
    if cfg.get("stop") is None or cfg.get("stop") == "all":
        moe()
        with nc.allow_non_contiguous_dma(reason="partition-broadcast param loads"):
            k.dma("sp", gnw[:], fnorm_d.partition_broadcast(128), reads=[R_gnw], writes=[R_gnw])
        for r_i in range(len(reqs)):
            combine(r_i)
        out_regs.append(R_yout)

    k.finish(out_regs)
    return nc


def _ret_consts():
    j = np.arange(128)[:, None].astype(np.float32)
    i = np.arange(128)[None, :].astype(np.float32)
    p = np.arange(128, dtype=np.float32)[:, None]
    return np.concatenate([np.maximum(i - j, 0), np.maximum(j - i, 0), (i >= j).astype(np.float32),
                           (j > i).astype(np.float32), p + 1, 128 - p, 127 - p, p], 1).astype(np.float32)


def _rope_tabs():
    t = np.arange(1024)
    row, col = t // 64, t % 64
    d = np.arange(128)
    freq = (10000.0 ** (-(np.arange(32, dtype=np.float32)) / 32)).astype(np.float32)
    pos = np.where((d < 64)[:, None], row[None, :], col[None, :]).astype(np.float32)
    ang = (pos * freq[d % 32][:, None]).astype(np.float32)
    sgn = np.where((d % 64) < 32, -1.0, 1.0)[:, None]
    return np.concatenate([np.cos(ang), np.sin(ang) * sgn], 1).astype(np.float32)


def _rope_psw():
    P = np.zeros((128, 128), np.float32)
    for d in range(128):
        P[d + 32 if (d % 64) < 32 else d - 32, d] = 1.0
    return P


def _s5_rowmask():
    par = (np.arange(128) // 16) % 2
    m = np.zeros((128, 4), np.float32)
    m[:, 0] = (par == 0)
    m[:, 1] = (par == 1)
    m[:, 2] = -m[:, 0]
    m[:, 3] = -m[:, 1]
    return m


def _moe_consts():
    m = np.zeros((128, 384), np.float32)
    m[:, 0:128] = np.arange(128, dtype=np.float32)[None, :]
    m[:, 128:256] = 1.0
    m[:, 256:384] = (np.arange(128)[:, None] < np.arange(128)[None, :]).astype(np.float32)
    return m


def kernel(**inputs):
    f = {k_: np.ascontiguousarray(np.asarray(v)) for k_, v in inputs.items()}
    B = f["x_prompt"].shape[0]
    BS = f["x_sample"].shape[0]
    npr = B // NCORES
    assert BS == NCORES and npr == 2
    TP, TS = f["x_prompt"].shape[1], f["x_sample"].shape[1]
    reqs = [(TP, False, 0, TP * i) for i in range(npr)] + [(TS, True, 1, TP * npr)]
    cfg = dict(reqs=reqs, ncv=2, n_prompt=npr)
    nc = build(cfg)
    common = dict(ident=np.eye(128, dtype=np.float32), w_ada=f["w_ada"][0], b_ada=f["b_ada"][0],
                  norm1=f["norm1"][0], norm2=f["norm2"][0], final_norm=f["final_norm"], w_in=f["w_in"][0],
                  ret_decay_logit=f["ret_decay_logit"][0].reshape(16), ret_gn_w=f["ret_gn_w"][0],
                  ret_consts=_ret_consts(), rope_tabs=_rope_tabs(), rope_psw=_rope_psw(),
                  s5_a_re=f["s5_a_re"][0], s5_a_im=f["s5_a_im"][0], s5_log_dt=f["s5_log_dt"][0], s5_b_re=f["s5_b_re"][0],
                  s5_b_im=f["s5_b_im"][0], s5_c_re=f["s5_c_re"][0], s5_c_im=f["s5_c_im"][0], s5_d=f["s5_d"][0],
                  s5_rowmask=_s5_rowmask(), w_s5_glu=f["w_s5_glu"][0], w_ret_out=f["w_ret_out"][0], w_out=f["w_out"][0],
                  w_router=f["w_router"][0], moe_consts=_moe_consts(),
                  w_exp_gate=f["w_exp_gate"][0], w_exp_up=f["w_exp_up"][0], w_exp_down=f["w_exp_down"][0])
    in_maps = []
    for c in range(NCORES):
        m = dict(common)
        m["xin"] = np.concatenate([f["x_prompt"][c * npr:(c + 1) * npr].reshape(npr * TP, D), f["x_sample"][c]], 0)
        cv = np.stack([f["c_ctx"], f["c"][c]], 0)
        m["cT"] = np.ascontiguousarray(cv.T.reshape(16, 128, 2).transpose(1, 0, 2).reshape(128, 32))
        m["state_ret"] = f["state_ret"][c, 0]
        m["state_s5_re"] = f["state_s5_re"][c, 0]
        m["state_s5_im"] = f["state_s5_im"][c, 0]
        in_maps.append(m)
    res = run_bass_kernel_spmd(nc, in_maps, core_ids=list(range(NCORES)))
    ys = [r["y"] for r in res.results]
    y_prompt = np.concatenate([y[:npr * TP].reshape(npr, TP, D) for y in ys], 0)
    y_sample = np.stack([y[npr * TP:] for y in ys], 0)
    new_re = np.concatenate([r["new_s5_re"] for r in res.results], 0)[:, None]
    new_im = np.concatenate([r["new_s5_im"] for r in res.results], 0)[:, None]
    new_ret = np.concatenate([r["new_ret"] for r in res.results], 0)[:, None]
    return (y_prompt.astype(np.float32), y_sample.astype(np.float32), new_re.astype(np.float32),
            new_im.astype(np.float32), new_ret.astype(np.float32))
```
